# Optimizing a Trainium2 kernel written in Bass

```python
import jax
import jax.numpy as jnp
from jax import lax
import numpy as np

D_MODEL = 1024
BATCH = 16
SEQ = 2048
DEPTH = 1

FOX_HEADS = 8
FOX_HEAD_DIM = 64
FOX_WIDTH = FOX_HEADS * FOX_HEAD_DIM
Q_BLOCK = 128
FORGET_BIAS_INIT = 5.0
GMLP_GROUPS = 8
GMLP_GROUP_DIM = 64
GMLP_WIDTH = GMLP_GROUPS * GMLP_GROUP_DIM
GMLP_CHUNK = 128
N_EXPERTS = 32
TOP_K = 4
D_FF = D_MODEL
SWIGLU_LIMIT = 7.0
SWIGLU_ALPHA = 1.702
MOE_BLOCK = 128
PLE_DIM = 256
LN_EPS = 1e-5
DEEPNORM_ALPHA = (2.0 * DEPTH) ** 0.25
DEEPNORM_BETA = (8.0 * DEPTH) ** -0.25

OFF_Q = 0
OFF_K = OFF_Q + FOX_WIDTH
OFF_V = OFF_K + FOX_WIDTH
OFF_F = OFF_V + FOX_WIDTH
OFF_U = OFF_F + FOX_HEADS
OFF_GV = OFF_U + GMLP_WIDTH
OFF_GA = OFF_GV + GMLP_WIDTH
OFF_GB = OFF_GA + D_MODEL
D_IN_PROJ = OFF_GB + D_MODEL
SPLIT_POINTS = (OFF_K, OFF_V, OFF_F, OFF_U, OFF_GV, OFF_GA, OFF_GB)

kernel_name = 'hybrid_fox_gmlp_moe_block'


def layer_norm(x, g, b):
    xf = x.astype(jnp.float32)
    mu = jnp.mean(xf, axis=-1, keepdims=True)
    var = jnp.mean(jnp.square(xf - mu), axis=-1, keepdims=True)
    return ((xf - mu) * lax.rsqrt(var + LN_EPS) * g + b).astype(x.dtype)


def forgetting_attention(q, k, v, f_logit):
    B, S, H, Dh = q.shape
    c = jnp.cumsum(jax.nn.log_sigmoid(f_logit.astype(jnp.float32)), axis=1)
    c = jnp.transpose(c, (0, 2, 1))
    q = jnp.transpose(q, (0, 2, 1, 3))
    k = jnp.transpose(k, (0, 2, 1, 3))
    v = jnp.transpose(v, (0, 2, 1, 3))
    scale = Dh ** -0.5
    outs = []
    for blk in range(S // Q_BLOCK):
        q0 = blk * Q_BLOCK
        kv_len = q0 + Q_BLOCK
        qb = q[:, :, q0:kv_len]
        kb = k[:, :, :kv_len]
        vb = v[:, :, :kv_len]
        logits = jnp.einsum('bhtd,bhsd->bhts', qb, kb).astype(jnp.float32) * scale
        logits = logits + c[:, :, q0:kv_len, None] - c[:, :, None, :kv_len]
        causal = jnp.arange(kv_len)[None, :] <= (q0 + jnp.arange(Q_BLOCK))[:, None]
        logits = jnp.where(causal, logits, -jnp.inf)
        probs = jax.nn.softmax(logits, axis=-1).astype(v.dtype)
        outs.append(jnp.einsum('bhts,bhsd->bthd', probs, vb))
    return jnp.concatenate(outs, axis=1).reshape(B, S, H * Dh)


def spatial_gating(u, v, ln_g, ln_b, w_s, b_s):
    B, S, _ = u.shape
    u = jax.nn.gelu(u)
    v = layer_norm(jax.nn.gelu(v), ln_g, ln_b)
    v = v.reshape(B, S // GMLP_CHUNK, GMLP_CHUNK, GMLP_GROUPS, GMLP_GROUP_DIM)
    w = w_s * jnp.tril(jnp.ones((GMLP_CHUNK, GMLP_CHUNK), w_s.dtype))
    s = jnp.einsum('gts,bnsgd->bntgd', w, v) + b_s.T[None, None, :, :, None]
    return u * s.reshape(B, S, GMLP_WIDTH)


def expert_ffn(xb, w_gu, b_gu, w_dn, b_dn):
    gu = xb @ w_gu + b_gu
    gate = jnp.minimum(gu[..., :D_FF], SWIGLU_LIMIT)
    up = jnp.clip(gu[..., D_FF:], -SWIGLU_LIMIT, SWIGLU_LIMIT)
    h = (up + 1.0) * (gate * jax.nn.sigmoid(SWIGLU_ALPHA * gate))
    return h @ w_dn + b_dn


def moe_ffn(x, w_router, b_router, w_gu, b_gu, w_dn, b_dn):
    B, S, D = x.shape
    T = B * S
    n_assign = T * TOP_K
    xf = x.reshape(T, D)
    logits = xf.astype(jnp.float32) @ w_router.astype(jnp.float32) + b_router.astype(jnp.float32)
    top_val, top_idx = lax.top_k(logits, TOP_K)
    gates = jax.nn.softmax(top_val, axis=-1).astype(x.dtype)
    e_flat = top_idx.reshape(-1).astype(jnp.int32)
    tok_flat = jnp.arange(n_assign, dtype=jnp.int32) // TOP_K
    order = jnp.argsort(e_flat)
    e_sorted = e_flat[order]
    tok_sorted = tok_flat[order]
    g_sorted = gates.reshape(-1)[order]
    counts = jnp.bincount(e_flat, length=N_EXPERTS).astype(jnp.int32)
    starts = jnp.cumsum(counts) - counts
    padded = (counts + MOE_BLOCK - 1) // MOE_BLOCK * MOE_BLOCK
    pad_end = jnp.cumsum(padded)
    pad_start = pad_end - padded
    rank = jnp.arange(n_assign, dtype=jnp.int32) - starts[e_sorted]
    dest = pad_start[e_sorted] + rank
    n_blocks = -(-n_assign // MOE_BLOCK) + N_EXPERTS
    x_pad = jnp.zeros((n_blocks * MOE_BLOCK, D), x.dtype).at[dest].set(xf[tok_sorted])
    block_start = jnp.arange(n_blocks, dtype=jnp.int32) * MOE_BLOCK
    block_expert = jnp.minimum(jnp.searchsorted(pad_end, block_start, side='right'), N_EXPERTS - 1)

    def run_block(args):
        xb, e = args
        return expert_ffn(xb, w_gu[e], b_gu[e], w_dn[e], b_dn[e])

    y_pad = lax.map(run_block, (x_pad.reshape(n_blocks, MOE_BLOCK, D), block_expert))
    y_sorted = y_pad.reshape(-1, D)[dest] * g_sorted[:, None]
    return jax.ops.segment_sum(y_sorted, tok_sorted, num_segments=T).reshape(B, S, D)


def setup_inputs(seed: int = 0) -> dict:
    key = jax.random.key(seed)
    ks = jax.random.split(key, 32)
    f32 = jnp.float32
    L = DEPTH

    def nrm(k, shape, scale):
        return scale * jax.random.normal(k, shape, f32)

    col_scale = jnp.ones((D_IN_PROJ,), f32).at[OFF_V:OFF_F].set(DEEPNORM_BETA)
    return {
        'x': nrm(ks[0], (BATCH, SEQ, D_MODEL), 1.0),
        'p': nrm(ks[1], (DEPTH, BATCH, SEQ, PLE_DIM), 1.0),
        'w_in': nrm(ks[2], (L, D_MODEL, D_IN_PROJ), D_MODEL ** -0.5) * col_scale,
        'b_in': nrm(ks[3], (L, D_IN_PROJ), 0.02).at[:, OFF_F:OFF_U].add(FORGET_BIAS_INIT),
        'gmlp_ln_g': 1.0 + nrm(ks[4], (L, GMLP_WIDTH), 0.02),
        'gmlp_ln_b': nrm(ks[5], (L, GMLP_WIDTH), 0.02),
        'w_spatial': nrm(ks[6], (L, GMLP_GROUPS, GMLP_CHUNK, GMLP_CHUNK), GMLP_CHUNK ** -0.5),
        'b_spatial': 1.0 + nrm(ks[7], (L, GMLP_GROUPS, GMLP_CHUNK), 0.02),
        'w_branch_a': nrm(ks[8], (L, FOX_WIDTH, D_MODEL), FOX_WIDTH ** -0.5 * DEEPNORM_BETA),
        'w_branch_b': nrm(ks[9], (L, GMLP_WIDTH, D_MODEL), GMLP_WIDTH ** -0.5 * DEEPNORM_BETA),
        'w_out': nrm(ks[10], (L, D_MODEL, D_MODEL), D_MODEL ** -0.5 * DEEPNORM_BETA),
        'b_out': nrm(ks[11], (L, D_MODEL), 0.02),
        'ln1_g': 1.0 + nrm(ks[12], (L, D_MODEL), 0.02),
        'ln1_b': nrm(ks[13], (L, D_MODEL), 0.02),
        'w_router': nrm(ks[14], (L, D_MODEL, N_EXPERTS), D_MODEL ** -0.5),
        'b_router': nrm(ks[15], (L, N_EXPERTS), 0.01),
        'w_gate_up': nrm(ks[16], (L, N_EXPERTS, D_MODEL, 2 * D_FF), D_MODEL ** -0.5),
        'b_gate_up': nrm(ks[17], (L, N_EXPERTS, 2 * D_FF), 0.02),
        'w_down': nrm(ks[18], (L, N_EXPERTS, D_FF, D_MODEL), D_FF ** -0.5 * DEEPNORM_BETA),
        'b_down': nrm(ks[19], (L, N_EXPERTS, D_MODEL), 0.02),
        'ln2_g': 1.0 + nrm(ks[20], (L, D_MODEL), 0.02),
        'ln2_b': nrm(ks[21], (L, D_MODEL), 0.02),
        'w_ple': nrm(ks[22], (L, PLE_DIM, D_MODEL), PLE_DIM ** -0.5 * DEEPNORM_BETA),
        'w_ple_gate': nrm(ks[23], (L, D_MODEL, D_MODEL), D_MODEL ** -0.5),
        'b_ple_gate': nrm(ks[24], (L, D_MODEL), 0.02),
        'ln3_g': 1.0 + nrm(ks[25], (L, D_MODEL), 0.02),
        'ln3_b': nrm(ks[26], (L, D_MODEL), 0.02),
    }


def reference(x, p, w_in, b_in, gmlp_ln_g, gmlp_ln_b, w_spatial, b_spatial,
              w_branch_a, w_branch_b, w_out, b_out, ln1_g, ln1_b,
              w_router, b_router, w_gate_up, b_gate_up, w_down, b_down,
              ln2_g, ln2_b, w_ple, w_ple_gate, b_ple_gate, ln3_g, ln3_b):
    h = x
    B, S, _ = h.shape
    for i in range(DEPTH):
        proj = h @ w_in[i] + b_in[i]
        q, k, v, f_logit, u, gv, gate_a, gate_b = jnp.split(proj, SPLIT_POINTS, axis=-1)
        attn = forgetting_attention(q.reshape(B, S, FOX_HEADS, FOX_HEAD_DIM),
                                    k.reshape(B, S, FOX_HEADS, FOX_HEAD_DIM),
                                    v.reshape(B, S, FOX_HEADS, FOX_HEAD_DIM),
                                    f_logit)
        sgu = spatial_gating(u, gv, gmlp_ln_g[i], gmlp_ln_b[i], w_spatial[i], b_spatial[i])
        merged = (jax.nn.sigmoid(gate_a) * (attn @ w_branch_a[i])
                  + jax.nn.sigmoid(gate_b) * (sgu @ w_branch_b[i]))
        mix = merged @ w_out[i] + b_out[i]
        h = layer_norm(DEEPNORM_ALPHA * h + mix, ln1_g[i], ln1_b[i])
        ffn = moe_ffn(h, w_router[i], b_router[i], w_gate_up[i], b_gate_up[i], w_down[i], b_down[i])
        h = layer_norm(DEEPNORM_ALPHA * h + ffn, ln2_g[i], ln2_b[i])
        ple = (p[i] @ w_ple[i]) * jax.nn.sigmoid(h @ w_ple_gate[i] + b_ple_gate[i])
        h = layer_norm(DEEPNORM_ALPHA * h + ple, ln3_g[i], ln3_b[i])
    return h
```

```python
import numpy as np
import concourse.bass as bass
import concourse.mybir as mybir
from concourse.bass_utils import run_bass_kernel_spmd

F32 = mybir.dt.float32
BF16 = mybir.dt.bfloat16
AF = mybir.ActivationFunctionType
ALU = mybir.AluOpType
AX = mybir.AxisListType

NCORES = 8
D = 1024
S = 2048
NSEQ = 2
TOK = NSEQ * S
H = 8
NE = 32
ALPHA = float(2.0 ** 0.25)
EPS = 1e-5
SILU_K = 1.702
SILU_CAP = float(7.0 * SILU_K / (1.0 + np.exp(-7.0 * SILU_K)))
GELU_C = 1.5957691216057308


class St:
    __slots__ = ("w", "r", "sem", "semval")

    def __init__(self):
        self.w = None
        self.r = []
        self.sem = None
        self.semval = 0


class Tl:
    def __init__(self, h, st=None):
        self.h = h
        self.st = st if st is not None else St()

    def __getitem__(self, k):
        return self.h[k]


class Sub(Tl):
    def __init__(self, base, off, width):
        self.h, self.st, self.off, self.w = base.h, base.st, off, width

    def __getitem__(self, k):
        rows, cols = k
        a = (cols.start or 0) + self.off
        b = (cols.stop if cols.stop is not None else self.w) + self.off
        return self.h[rows, a:b]


class Op:
    __slots__ = ("eng", "fn", "deps", "inc", "val", "dma", "sem")


class Prog:
    ENG = ("pe", "act", "dve", "pool", "sp")

    def __init__(self, nc):
        self.nc = nc
        self.ops = []
        self.pending = {}
        self.last = {}
        self.dmas_since = []
        self.stores = []
        self.nsem = 0

    def op(self, eng, fn, reads=(), writes=(), dma=None, extra=()):
        o = Op()
        o.eng, o.fn, o.inc, o.val, o.dma, o.sem = eng, fn, False, 0, dma, None
        deps = set(extra)
        war = set()
        for t in reads:
            if t.st.w is not None:
                deps.add(t.st.w)
        for t in writes:
            if t.st.w is not None:
                deps.add(t.st.w)
            for rr in t.st.r:
                war.add(rr)
        for t in reads:
            if dma is None:
                t.st.r = [x for x in t.st.r if x.dma is not None or x.eng != eng]
            t.st.r.append(o)
        for t in writes:
            t.st.w = o
            t.st.r = []
        for d in war:
            deps.add(d)
        if eng == "pe" and dma is None:
            deps = {d for d in deps if d.dma is not None or d.eng != "pe"}
        if eng in self.pending:
            deps |= self.pending.pop(eng)
        deps.discard(o)
        o.deps = deps
        if dma is not None:
            st = dma.st
            if st.sem is None:
                st.sem = self.nc.alloc_semaphore("dsem%d" % self.nsem)
                self.nsem += 1
            st.semval += 16
            o.sem, o.val = st.sem, st.semval
            self.dmas_since.append(o)
        self.last[eng] = o
        self.ops.append(o)
        return o

    def barrier(self):
        deps = set(self.last.values()) | set(self.dmas_since)
        self.dmas_since = []
        for e in self.ENG:
            self.pending[e] = set(deps)

    def emit(self):
        nc = self.nc
        esem = {e: nc.alloc_semaphore("esem_" + e) for e in self.ENG}
        fin = self.op("sp", None, extra=list(self.stores))
        for o in self.ops:
            for d in o.deps:
                d.inc = True
        cnt = {e: 0 for e in self.ENG}
        for o in self.ops:
            if o.dma is None and o.inc:
                cnt[o.eng] += 1
                o.val = cnt[o.eng]
                o.sem = esem[o.eng]
        byeng = {e: [o for o in self.ops if o.eng == e] for e in self.ENG}

        def run(eng_name, eng):
            waited = {}
            for o in byeng[eng_name]:
                need = {}
                for d in o.deps:
                    k = id(d.sem)
                    if k not in need or need[k][1] < d.val:
                        need[k] = (d.sem, d.val)
                for k, (sem, val) in need.items():
                    if waited.get(k, 0) < val:
                        eng.wait_ge(sem, val)
                        waited[k] = val
                if o.fn is None:
                    continue
                ins = o.fn(eng)
                if o.dma is not None:
                    ins.then_inc(o.sem, 16)
                elif o.inc:
                    ins.then_inc(o.sem, 1)

        with nc.Block() as block:
            @block.tensor
            def _(e):
                run("pe", e)

            @block.scalar
            def _(e):
                run("act", e)

            @block.vector
            def _(e):
                run("dve", e)

            @block.gpsimd
            def _(e):
                run("pool", e)

            @block.sync
            def _(e):
                run("sp", e)


class Mem:
    BASE = 16512
    TOP = 229344

    def __init__(self, nc):
        self.nc = nc
        self.cur = self.BASE
        self.n = 0

    def alloc(self, shape, dtype, st=None):
        nb = 1
        for s_ in shape[1:]:
            nb *= s_
        nb *= 2 if dtype == BF16 else 4
        nb = (nb + 31) // 32 * 32
        assert self.cur + nb <= self.TOP, ("SBUF overflow", self.cur, nb)
        h = self.nc.alloc_sbuf_tensor_at("sb%d" % self.n, list(shape), dtype, offset=self.cur)
        self.n += 1
        self.cur += nb
        return Tl(h, st)


def build(debug=False):
    nc = bass.Bass("TRN2", target_bir_lowering=False)
    P = Prog(nc)
    M = Mem(nc)

    def din(name, shape):
        return nc.dram_tensor(name, list(shape), F32, kind="ExternalInput").ap()

    x_tm = din("x_tm", [TOK, D])
    xT = din("xT", [8, 128, TOK])
    pT = din("pT", [2, 128, TOK])
    w_fm = din("w_fm", [8, 128, 3584])
    w_tm = din("w_tm", [8, 128, 1032])
    b_fm = din("b_fm", [128, 28])
    b_tm = din("b_tm", [128, 1032])
    lng = din("lng", [128, 512])
    lnb = din("lnb", [128, 512])
    wsT = din("wsT", [128, 8 * 128])
    bsp = din("bsp", [1, 8 * 128])
    w_a = din("w_a", [64, 8 * 1024])
    w_b = din("w_b", [128, 4 * 1024])
    w_o = din("w_o", [128, 8 * 1024])
    vecs = din("vecs", [128, 8 * 1024])
    w_r = din("w_r", [128, 8 * 32])
    b_r = din("b_r", [128, 32])
    wgu = din("wgu", [NE, 8, 128, 2048])
    bgu = din("bgu", [128, NE * 16])
    wdn = din("wdn", [NE, 128, 8192])
    bdn = din("bdn", [NE, 1, 1024])
    w_pl = din("w_pl", [128, 2 * 1024])
    w_pg = din("w_pg", [128, 8 * 1024])
    cst = din("cst", [128, 6 * 128])
    out = nc.dram_tensor("out", [TOK, D], F32, kind="ExternalOutput").ap()
    h1s_h = nc.dram_tensor("h1s", [TOK, D], F32, kind="ExternalOutput").ap()
    h1s = [Tl(h1s_h) for _ in range(TOK // 128)]

    ps = [Tl(nc.alloc_psum_tensor("ps%d" % i, [128, 512], F32)) for i in range(8)]
    psn = [0]

    def psum():
        psn[0] = (psn[0] + 1) % 6
        return ps[psn[0]]
    pan = [0]

    def psum_acc():
        pan[0] += 1
        return ps[6 + pan[0] % 2]

    def dma(q, dst, dst_ap, src_ap, src=None, sem=None):
        reads = [src] if src is not None else []
        o = P.op(q, lambda e: e.dma_start(out=dst_ap, in_=src_ap), reads=reads, writes=[dst],
                 dma=(sem if sem is not None else dst))
        return o

    def mm(pst, out_ap, lhsT, rhs, start, stop, reads):
        P.op("pe", lambda e: e.matmul(out_ap, lhsT, rhs, start=start, stop=stop, skip_group_check=True),
             reads=reads, writes=[pst])

    def act(out_t, out_ap, in_t, in_ap, func, bias=0.0, scale=1.0, extra_reads=()):
        P.op("act", lambda e: e.activation(out_ap, in_ap, func, bias=bias, scale=scale),
             reads=[in_t] + list(extra_reads), writes=[out_t])

    def ts(out_t, out_ap, in_t, in_ap, s1, s2, op0, op1=None, extra_reads=()):
        if op1 is None:
            P.op("dve", lambda e: e.tensor_scalar(out_ap, in_ap, s1, None, op0),
                 reads=[in_t] + list(extra_reads), writes=[out_t])
        else:
            P.op("dve", lambda e: e.tensor_scalar(out_ap, in_ap, s1, s2, op0, op1),
                 reads=[in_t] + list(extra_reads), writes=[out_t])

    def tt(out_t, out_ap, a_t, a_ap, b_t, b_ap, op):
        P.op("dve", lambda e: e.tensor_tensor(out_ap, a_ap, b_ap, op), reads=[a_t, b_t], writes=[out_t])

    def stt(out_t, out_ap, a_t, a_ap, scalar, b_t, b_ap, op0, op1, extra_reads=()):
        P.op("dve", lambda e: e.scalar_tensor_tensor(out_ap, a_ap, scalar, b_ap, op0, op1),
             reads=[a_t, b_t] + list(extra_reads), writes=[out_t])

    cst_f = M.alloc([128, 768], F32)
    cst_b = M.alloc([128, 768], BF16)
    dma("sp", cst_f, cst_f[:, :], cst[:, :])
    dma("pool", cst_b, cst_b[:, :], cst[:, :])
    ident_f = cst_f[:, 0:128]
    triu_f = cst_f[:, 128:256]
    last_f = cst_f[:, 256:384]
    triu_b = cst_b[:, 128:256]
    ones_b = cst_b[:, 384:512]
    ident_b = cst_b[:, 0:128]
    allones_b = cst_b[:, 384:512]
    sut_b = cst_b[:, 512:640]
    iota_f = cst_f[:, 640:768]
    mark0 = M.cur
    vec_t = M.alloc([128, 3 * 1024], F32)
    dma("sp", vec_t, vec_t[:, :], vecs[:, 0:3072])

    def vec(k):
        return vec_t[:, k * 1024:(k + 1) * 1024]

    def gelu(dst_t, dst_ap, src_t, src_ap, tmp_t, tmp_ap):
        tt(tmp_t, tmp_ap, src_t, src_ap, src_t, src_ap, ALU.mult)
        ts(tmp_t, tmp_ap, tmp_t, tmp_ap, 0.044715, 1.0, ALU.mult, ALU.add)
        tt(tmp_t, tmp_ap, tmp_t, tmp_ap, src_t, src_ap, ALU.mult)
        act(tmp_t, tmp_ap, tmp_t, tmp_ap, AF.Sigmoid, scale=GELU_C)
        tt(dst_t, dst_ap, tmp_t, tmp_ap, src_t, src_ap, ALU.mult)

    def layer_norm(dst_t, dst_ap, src_t, W, g_ap, b_ap, st_t, tmp_t, gb_t):
        nch = W // 512
        for ch in range(nch):
            P.op("dve", lambda e, ch=ch: e.bn_stats(st_t[:, 6 * ch:6 * ch + 6], src_t[:, 512 * ch:512 * ch + 512]),
                 reads=[src_t], writes=[st_t])
        P.op("dve", lambda e: e.bn_aggr(st_t[:, 16:18], st_t[:, 0:6 * nch]), reads=[st_t], writes=[st_t])
        act(st_t, st_t[:, 19:20], st_t, st_t[:, 17:18], AF.Ln, bias=EPS)
        act(st_t, st_t[:, 18:19], st_t, st_t[:, 19:20], AF.Exp, scale=-0.5)
        if W >= 1024:
            ts(st_t, st_t[:, 20:21], st_t, st_t[:, 16:17], st_t[:, 18:19], -1.0, ALU.mult, ALU.mult)
            act(tmp_t, tmp_t[:, 0:W], src_t, src_t[:, 0:W], AF.Identity, bias=st_t[:, 20:21], scale=st_t[:, 18:19],
                extra_reads=[st_t])
        else:
            ts(tmp_t, tmp_t[:, 0:W], src_t, src_t[:, 0:W], st_t[:, 16:17], st_t[:, 18:19], ALU.subtract, ALU.mult,
               extra_reads=[st_t])
        tt(tmp_t, tmp_t[:, 0:W], tmp_t, tmp_t[:, 0:W], gb_t, g_ap, ALU.mult)
        tt(dst_t, dst_ap, tmp_t, tmp_t[:, 0:W], gb_t, b_ap, ALU.add)

    RS = 6
    ring = [M.alloc([128, 4096], BF16) for _ in range(RS)]
    rn = [0]

    def wload(src_ap, view, npart=128):
        sl = ring[rn[0] % RS]
        rn[0] += 1
        dma("pool", sl, view(sl), src_ap)
        return sl

    kT = M.alloc([128, 4, S], BF16)
    vaug = M.alloc([128, 16, H * 65], BF16)
    cneg = M.alloc([128, 16 * 8], F32)
    lsb = M.alloc([128, 16 * 16], BF16)
    rcb = [M.alloc([128, 1024], BF16) for _ in range(2)]
    biasq = M.alloc([128, 16 * 8], F32)
    wsT_b = M.alloc([128, 8 * 128], BF16)
    bsp_b = M.alloc([1, 8 * 128], BF16)
    btm_t = M.alloc([128, 1032], F32)
    bfm_t = M.alloc([128, 28], F32)
    lngb = M.alloc([128, 1024], F32)
    XT = [M.alloc([128, 8, 512], BF16) for _ in range(2)]
    qT = M.alloc([128, 4, 512], BF16)
    uT = M.alloc([128, 4, 512], BF16)
    attnT = M.alloc([128, 8, 512], BF16)
    sguT = M.alloc([128, 4, 512], BF16)
    vn = M.alloc([128, 4, 512], BF16)
    mrgT = M.alloc([128, 8, 512], BF16)
    ta = [M.alloc([128, 512], F32) for _ in range(4)]
    ex = [M.alloc([128, 512], BF16) for _ in range(4)]
    exn = [0]
    smf = [M.alloc([128, 24], F32) for _ in range(4)]
    stq = [M.alloc([128, 32], F32) for _ in range(4)]
    rcp = [M.alloc([128, 512], F32) for _ in range(2)]
    otm = rcp
    xt = [M.alloc([128, 1024], F32) for _ in range(2)]
    zt = M.alloc([128, 1024], F32)
    zt2 = M.alloc([128, 1024], F32)
    zg = [Sub(zt, 0, 512), Sub(zt, 512, 512), Sub(zt2, 0, 512), Sub(zt2, 512, 512)]
    h1o = [M.alloc([128, 1024], F32) for _ in range(2)]
    stt_t = M.alloc([128, 32], F32)
    sm = M.alloc([128, 64], F32)

    bo_b = M.alloc([1, 2048], BF16)
    ts(bo_b, bo_b[0:1, 0:1024], vec_t, vec_t[0:1, 0:1024], 1.0, None, ALU.mult)
    tt(bo_b, bo_b[0:1, 1024:2048], vec_t, vec_t[0:1, 0:1024], bo_b, bo_b[0:1, 0:1024], ALU.subtract)
    dma("pool", wsT_b, wsT_b[:, :], wsT[:, :])
    dma("pool", bsp_b, bsp_b[:, :], bsp[:, :])
    dma("sp", btm_t, btm_t[:, :], b_tm[:, :])
    dma("sp", bfm_t, bfm_t[:, :], b_fm[:, :])
    dma("sp", lngb, lngb[:, 0:512], lng[:, :])
    dma("sp", lngb, lngb[:, 512:1024], lnb[:, :])
    for g in range(8):
        tt(wsT_b, wsT_b[:, g * 128:(g + 1) * 128], wsT_b, wsT_b[:, g * 128:(g + 1) * 128], cst_b, triu_b, ALU.mult)
    P.op("dve", lambda e: e.memset(vaug[:, :, :], 1.0), writes=[vaug])

    def v3(sl, c):
        return sl[:, :].rearrange("p (c n) -> p c n", c=c)

    xtn = [0]
    for s in range(NSEQ):
        for j in range(4):
            T0 = s * S + j * 512
            X = XT[xtn[0] % 2]
            xtn[0] += 1
            dma("pool", X, X[:, :, :], xT[:, :, T0:T0 + 512].rearrange("c p t -> p c t"))
            wv = wload(w_tm[:, :, 0:512].rearrange("c p n -> p c n"), lambda sl: v3(sl, 8))
            wg = wload(w_tm[:, :, 512:1024].rearrange("c p n -> p c n"), lambda sl: v3(sl, 8))
            wf = wload(w_tm[:, :, 1024:1032].rearrange("c p n -> p c n"), lambda sl: sl[:, 0:64].rearrange("p (c n) -> p c n", c=8))
            for i in range(4):
                ti = j * 4 + i
                pv, pg, pf = psum(), psum(), psum()
                for c in range(8):
                    lw = X[:, c, i * 128:(i + 1) * 128]
                    mm(pv, pv[:, :], lw, v3(wv, 8)[:, c, :], c == 0, c == 7, [X, wv])
                for c in range(8):
                    lw = X[:, c, i * 128:(i + 1) * 128]
                    mm(pg, pg[:, :], lw, v3(wg, 8)[:, c, :], c == 0, c == 7, [X, wg])
                for c in range(8):
                    lw = X[:, c, i * 128:(i + 1) * 128]
                    mm(pf, pf[:, 0:8], lw, wf[:, 0:64].rearrange("p (c n) -> p c n", c=8)[:, c, :], c == 0, c == 7, [X, wf])
                P.op("dve", lambda e, pv=pv, ti=ti: e.tensor_tensor(
                    vaug[:, ti, :].rearrange("p (h d) -> p h d", h=H)[:, :, 0:64],
                    pv[:, :].rearrange("p (h d) -> p h d", h=H),
                    btm_t[:, 0:512].rearrange("p (h d) -> p h d", h=H), ALU.add),
                    reads=[pv, btm_t], writes=[vaug])
                tt(zg[i], zg[i][:, :], pg, pg[:, :], btm_t, btm_t[:, 512:1024], ALU.add)
                tt(smf[i], smf[i][:, 0:8], pf, pf[:, 0:8], btm_t, btm_t[:, 1024:1032], ALU.add)
                act(smf[i], smf[i][:, 8:16], smf[i], smf[i][:, 0:8], AF.Exp, scale=-1.0)
                act(smf[i], smf[i][:, 16:24], smf[i], smf[i][:, 8:16], AF.Ln, bias=1.0)

            def tm_tail(j=j):
                for i in range(4):
                    ti = j * 4 + i
                    ts(lsb, lsb[:, ti * 16:ti * 16 + 8], smf[i], smf[i][:, 16:24], 1.0, None, ALU.mult)
                    tt(lsb, lsb[:, ti * 16 + 8:ti * 16 + 16], smf[i], smf[i][:, 16:24], lsb, lsb[:, ti * 16:ti * 16 + 8],
                       ALU.subtract)
                for i in range(4):
                    ti = j * 4 + i
                    pc = psum()
                    for t2 in range(ti):
                        mm(pc, pc[:, 0:8], allones_b, lsb[:, t2 * 16:t2 * 16 + 8], t2 == 0, False, [cst_b, lsb])
                        mm(pc, pc[:, 0:8], allones_b, lsb[:, t2 * 16 + 8:t2 * 16 + 16], False, False, [cst_b, lsb])
                    mm(pc, pc[:, 0:8], triu_b, lsb[:, ti * 16:ti * 16 + 8], ti == 0, False, [cst_b, lsb])
                    mm(pc, pc[:, 0:8], triu_b, lsb[:, ti * 16 + 8:ti * 16 + 16], False, True, [cst_b, lsb])
                    P.op("act", lambda e, pc=pc, ti=ti: e.activation(cneg[:, ti * 8:(ti + 1) * 8], pc[:, 0:8], AF.Copy),
                         reads=[pc], writes=[cneg])

            def tm_gv():
                for i in range(4):
                    gelu(zg[i], zg[i][:, :], zg[i], zg[i][:, :], ta[i % 2], ta[i % 2][:, :])
                for i in range(4):
                    layer_norm(vn, vn[:, i, :], zg[i], 512, lngb[:, 0:512], lngb[:, 512:1024], stq[i], ta[2 + i % 2], lngb)
            def tm_bias(j=j):
              tref = j * 4 + 3
              pr = psum()
              for t2 in range(tref + 1):
                  mm(pr, pr[:, 0:8], allones_b, lsb[:, t2 * 16:t2 * 16 + 8], t2 == 0, False, [cst_b, lsb])
                  mm(pr, pr[:, 0:8], allones_b, lsb[:, t2 * 16 + 8:t2 * 16 + 16], False, t2 == tref, [cst_b, lsb])
              act(sm, sm[:, 24:32], pr, pr[:, 0:8], AF.Copy)
              for ti in range(tref + 1):
                tt(biasq, biasq[:, ti * 8:(ti + 1) * 8], cneg, cneg[:, ti * 8:(ti + 1) * 8], sm, sm[:, 24:32], ALU.subtract)
            tref_unused = j * 4 + 3
            for blk in range(3):
                wp = wload(w_fm[:, :, blk * 512:(blk + 1) * 512].rearrange("c p n -> p c n"), lambda sl: v3(sl, 8))
                for m in range(4):
                    pq = psum()
                    for c in range(8):
                        mm(pq, pq[:, :], v3(wp, 8)[:, c, m * 128:(m + 1) * 128], X[:, c, :], c == 0, c == 7, [wp, X])
                    bcol = bfm_t[:, blk * 4 + m:blk * 4 + m + 1]
                    if blk == 0:
                        act(qT, qT[:, m, :], pq, pq[:, :], AF.Identity, bias=bcol, extra_reads=[bfm_t])
                    elif blk == 1:
                        act(kT, kT[:, m, j * 512:(j + 1) * 512], pq, pq[:, :], AF.Identity, bias=bcol, extra_reads=[bfm_t])
                    else:
                        t0_ = ta[2 + m % 2]
                        act(t0_, t0_[:, :], pq, pq[:, :], AF.Identity, bias=bcol, extra_reads=[bfm_t])
                        gelu(uT, uT[:, m, :], t0_, t0_[:, :], ta[m % 2], ta[m % 2][:, :])
                if blk == 0:
                    tm_tail()
                    tm_bias()
                if blk == 1:
                    tm_gv()
            pend_tail = []
            for h in range(H):
                pcn, pb = h // 2, (h % 2) * 64
                po = psum_acc()
                nkt = 4 * j + 4
                live = {}

                def issue_s(ki, pcn=pcn, pb=pb):
                    dj = ki - 4 * j
                    q0 = 0 if dj < 0 else dj * 128
                    pS = psum()
                    mm(pS, pS[:, q0:512], kT[pb:pb + 64, pcn, ki * 128:(ki + 1) * 128], qT[pb:pb + 64, pcn, q0:512],
                       True, True, [kT, qT])
                    live[ki] = (pS, q0, dj)
                issue_s(0)
                if nkt > 1:
                    issue_s(1)
                if pend_tail:
                    pend_tail.pop(0)()
                for ki in range(nkt):
                    pS, q0, dj = live.pop(ki)
                    E = ex[exn[0] % 4]
                    exn[0] += 1
                    act(E, E[:, q0:512], pS, pS[:, q0:512], AF.Exp, bias=biasq[:, ki * 8 + h:ki * 8 + h + 1],
                        scale=0.125, extra_reads=[biasq])
                    if dj >= 0:
                        tt(E, E[:, q0:q0 + 128], E, E[:, q0:q0 + 128], cst_b, triu_b, ALU.mult)
                    if ki + 2 < nkt:
                        issue_s(ki + 2)
                    mm(po, po[0:65, q0:512], vaug[:, ki, h * 65:(h + 1) * 65], E[:, q0:512], ki == 0, ki == nkt - 1,
                       [vaug, E])

                def tail(po=po, h=h):
                    rc, ot = rcp[h % 2], otm[h % 2]
                    act(rc, rc[64:65, :], po, po[64:65, :], AF.Ln)
                    act(rc, rc[64:65, :], rc, rc[64:65, :], AF.Exp, scale=-1.0)
                    rb = rcb[h % 2]
                    ts(rb, rb[64:65, 0:512], rc, rc[64:65, :], 1.0, None, ALU.mult)
                    tt(rb, rb[64:65, 512:1024], rc, rc[64:65, :], rb, rb[64:65, 0:512], ALU.subtract)
                    pbc = psum()
                    mm(pbc, pbc[0:64, :], cst_b[64:65, 384:448], rb[64:65, 0:512], True, False, [cst_b, rb])
                    mm(pbc, pbc[0:64, :], cst_b[64:65, 384:448], rb[64:65, 512:1024], False, True, [cst_b, rb])
                    act(ot, ot[0:64, :], po, po[0:64, :], AF.Copy)
                    tt(attnT, attnT[0:64, h, :], ot, ot[0:64, :], pbc, pbc[0:64, :], ALU.mult)
                pend_tail.append(tail)
            pend_tail.pop(0)()
            for g in range(8):
                pcn, pb = g // 2, (g % 2) * 64
                pg_ = psum()
                for i in range(4):
                    mm(pg_, pg_[:, i * 128:(i + 1) * 128], vn[:, i, pcn * 128:(pcn + 1) * 128],
                       wsT_b[:, g * 128:(g + 1) * 128], True, False, [vn, wsT_b])
                    mm(pg_, pg_[:, i * 128:(i + 1) * 128], ones_b[0:1, :], bsp_b[0:1, g * 128:(g + 1) * 128],
                       False, True, [cst_b, bsp_b])
                tt(sguT, sguT[pb:pb + 64, pcn, :], uT, uT[pb:pb + 64, pcn, :], pg_, pg_[pb:pb + 64, :], ALU.mult)
            wa0 = wload(w_a[:, :].rearrange("p (h n) -> p h n", h=8)[:, :, 0:512], lambda sl: sl[0:64, :].rearrange("p (h n) -> p h n", h=8))
            wa1 = wload(w_a[:, :].rearrange("p (h n) -> p h n", h=8)[:, :, 512:1024], lambda sl: sl[0:64, :].rearrange("p (h n) -> p h n", h=8))
            wb_ = wload(w_b[:, :], lambda sl: sl[:, :])
            for mb in range(2):
                wga = wload(w_fm[:, :, 1536 + mb * 512:1536 + (mb + 1) * 512].rearrange("c p n -> p c n"), lambda sl: v3(sl, 8))
                wgb = wload(w_fm[:, :, 2560 + mb * 512:2560 + (mb + 1) * 512].rearrange("c p n -> p c n"), lambda sl: v3(sl, 8))
                wa = wa0 if mb == 0 else wa1
                for mi in range(4):
                    m = mb * 4 + mi
                    pGA, pGB, pA, pB = psum(), psum(), psum(), psum()
                    for c in range(8):
                        mm(pGA, pGA[:, :], v3(wga, 8)[:, c, mi * 128:(mi + 1) * 128], X[:, c, :], c == 0, c == 7, [wga, X])
                    for c in range(8):
                        mm(pGB, pGB[:, :], v3(wgb, 8)[:, c, mi * 128:(mi + 1) * 128], X[:, c, :], c == 0, c == 7, [wgb, X])
                    for h in range(8):
                        mm(pA, pA[:, :], wa[0:64, :].rearrange("p (h n) -> p h n", h=8)[:, h, mi * 128:(mi + 1) * 128],
                           attnT[0:64, h, :], h == 0, h == 7, [wa, attnT])
                    for c in range(4):
                        mm(pB, pB[:, :], wb_[:, c * 1024 + m * 128:c * 1024 + (m + 1) * 128], sguT[:, c, :], c == 0, c == 3,
                           [wb_, sguT])
                    g0_, g1_ = ta[(m % 2) * 2], ta[(m % 2) * 2 + 1]
                    act(g0_, g0_[:, :], pGA, pGA[:, :], AF.Sigmoid, bias=bfm_t[:, 12 + m:13 + m], extra_reads=[bfm_t])
                    act(g1_, g1_[:, :], pGB, pGB[:, :], AF.Sigmoid, bias=bfm_t[:, 20 + m:21 + m], extra_reads=[bfm_t])
                    tt(g0_, g0_[:, :], g0_, g0_[:, :], pA, pA[:, :], ALU.mult)
                    tt(g1_, g1_[:, :], g1_, g1_[:, :], pB, pB[:, :], ALU.mult)
                    tt(mrgT, mrgT[:, m, :], g0_, g0_[:, :], g1_, g1_[:, :], ALU.add)
            wo0 = wload(w_o[:, 0:4096], lambda sl: sl[:, :])
            wo1 = wload(w_o[:, 4096:8192], lambda sl: sl[:, :])
            for i in range(4):
                ti = (T0 // 128) + i
                xx = xt[ti % 2]
                dma("sp", xx, xx[:, :], x_tm[ti * 128:(ti + 1) * 128, :])
                for hf in range(2):
                    pz = psum()
                    for c in range(8):
                        wo = wo0 if c < 4 else wo1
                        cc = c % 4
                        mm(pz, pz[:, :], mrgT[:, c, i * 128:(i + 1) * 128],
                           wo[:, cc * 1024 + hf * 512:cc * 1024 + (hf + 1) * 512], c == 0, False, [mrgT, wo])
                    mm(pz, pz[:, :], ones_b[0:1, :], bo_b[0:1, hf * 512:(hf + 1) * 512], False, False, [cst_b, bo_b])
                    mm(pz, pz[:, :], ones_b[0:1, :], bo_b[0:1, 1024 + hf * 512:1024 + (hf + 1) * 512], False, True, [cst_b, bo_b])
                    stt(zt2, zt2[:, hf * 512:(hf + 1) * 512], xx, xx[:, hf * 512:(hf + 1) * 512], ALPHA, pz, pz[:, :],
                        ALU.mult, ALU.add)
                ho = h1o[ti % 2]
                layer_norm(ho, ho[:, :], zt2, 1024, vec(1), vec(2), stt_t, zt, vec_t)
                dma("sp", h1s[ti], h1s_h[ti * 128:(ti + 1) * 128, :], ho[:, :], src=ho, sem=ho)

    for s in range(NSEQ):
        P.barrier()
        M.cur = mark0
        acc = [M.alloc([128, 1024], F32) for _ in range(16)]
        markc = M.cur
        h1b = [M.alloc([128, 1024], BF16) for _ in range(16)]
        selp = M.alloc([128, 16 * 32], F32)
        gath = M.alloc([128, 16 * 32], BF16)
        gatl = M.alloc([128, 16 * 32], BF16)
        gsl = [M.alloc([128, 4], F32) for _ in range(2)]
        wr_b = M.alloc([128, 8 * 32], BF16)
        br_t = M.alloc([128, 32], F32)
        bgu_t = M.alloc([128, NE * 16], F32)
        bgk_t = M.alloc([128, NE * 16], F32)
        wgr = [M.alloc([128, 2, 8, 128], BF16) for _ in range(4)]
        wdr = [M.alloc([128, 8, 1024], BF16) for _ in range(1)]
        bdf = [M.alloc([1, 1024], F32) for _ in range(1)]
        bdb = [M.alloc([1, 1024], BF16) for _ in range(2)]
        sm2s = [M.alloc([128, 128], F32) for _ in range(2)]
        markh = M.cur
        hst = [M.alloc([128, 1024], F32) for _ in range(2)]
        hTt = [M.alloc([128, 8, 128], BF16) for _ in range(2)]
        gat = M.alloc([128, 16 * 32], F32)
        maskb = M.alloc([128, 16 * 32], BF16)
        dma("pool", wr_b, wr_b[:, :], w_r[:, :])
        dma("sp", br_t, br_t[:, :], b_r[:, :])
        dma("sp", bgu_t, bgu_t[:, :], bgu[:, :])
        act(bgk_t, bgk_t[:, :], bgu_t, bgu_t[:, :], AF.Copy, scale=SILU_K)

        pls = {}

        def m0_a(i):
            ti = s * 16 + i
            hs = hst[i % 2]
            hT = hTt[i % 2]
            dma("sp", hs, hs[:, :], h1s_h[ti * 128:(ti + 1) * 128, :], src=h1s[ti])
            act(acc[i], acc[i][:, :], hs, hs[:, :], AF.Copy, scale=ALPHA)
            act(h1b[i], h1b[i][:, :], hs, hs[:, :], AF.Copy)
            for hf in range(2):
                pt_ = psum()
                for c4 in range(4):
                    c = hf * 4 + c4
                    mm(pt_, pt_[:, c4 * 128:(c4 + 1) * 128], h1b[i][:, c * 128:(c + 1) * 128], ident_b, True, True,
                       [h1b[i], cst_b])
                P.op("act", lambda e, pt_=pt_, hT=hT, hf=hf: e.activation(
                    hT[:, hf * 4:(hf + 1) * 4, :], pt_[:, :].rearrange("p (c t) -> p c t", c=4), AF.Copy),
                    reads=[pt_], writes=[hT])
            pl = psum()
            for c in range(8):
                mm(pl, pl[:, 0:32], hT[:, c, :], wr_b[:, c * 32:(c + 1) * 32], c == 0, c == 7, [hT, wr_b])
            pls[i] = pl

        def m0_b(i):
            pl = pls.pop(i)
            sm2 = sm2s[i % 2]
            tt(sm2, sm2[:, 0:32], pl, pl[:, 0:32], br_t, br_t[:, :], ALU.add)
            P.op("dve", lambda e: e.max(out=sm2[:, 32:40], in_=sm2[:, 0:32]), reads=[sm2], writes=[sm2])
            ts(sm2, sm2[:, 40:72], sm2, sm2[:, 0:32], sm2[:, 35:36], None, ALU.is_ge)
            ts(maskb, maskb[:, i * 32:(i + 1) * 32], sm2, sm2[:, 0:32], sm2[:, 35:36], None, ALU.is_ge)
            ts(sm2, sm2[:, 72:73], sm2, sm2[:, 32:33], -1.0, None, ALU.mult)
            act(sm2, sm2[:, 80:112], sm2, sm2[:, 0:32], AF.Exp, bias=sm2[:, 72:73])
            tt(sm2, sm2[:, 80:112], sm2, sm2[:, 80:112], sm2, sm2[:, 40:72], ALU.mult)
            P.op("dve", lambda e: e.reduce_sum(sm2[:, 73:74], sm2[:, 80:112], axis=AX.X), reads=[sm2], writes=[sm2])
            P.op("dve", lambda e: e.reciprocal(sm2[:, 74:75], sm2[:, 73:74]), reads=[sm2], writes=[sm2])
            ts(gat, gat[:, i * 32:(i + 1) * 32], sm2, sm2[:, 80:112], sm2[:, 74:75], 1.0 / SILU_K, ALU.mult, ALU.mult)
            ts(gath, gath[:, i * 32:(i + 1) * 32], gat, gat[:, i * 32:(i + 1) * 32], 1.0, None, ALU.mult)
            tt(gatl, gatl[:, i * 32:(i + 1) * 32], gat, gat[:, i * 32:(i + 1) * 32], gath, gath[:, i * 32:(i + 1) * 32],
               ALU.subtract)
            g0, k = (i // 4) * 4, i % 4
            prk = psum()
            for k2 in range(k):
                mm(prk, prk[:, 0:32], ones_b, maskb[:, (g0 + k2) * 32:(g0 + k2 + 1) * 32], k2 == 0, False, [cst_b, maskb])
            mm(prk, prk[:, 0:32], sut_b, maskb[:, i * 32:(i + 1) * 32], k == 0, True, [cst_b, maskb])
            stt(selp, selp[:, i * 32:(i + 1) * 32], prk, prk[:, 0:32], 1.0, sm2, sm2[:, 40:72], ALU.add, ALU.mult)
            ts(selp, selp[:, i * 32:(i + 1) * 32], selp, selp[:, i * 32:(i + 1) * 32], -1.0, None, ALU.add)

        m0_a(0)
        for i in range(16):
            if i + 1 < 16:
                m0_a(i + 1)
            m0_b(i)

        P.barrier()
        M.cur = markh
        tb = [M.alloc([128, 512], F32) for _ in range(4)]
        Pm = [M.alloc([128, 16, 128], BF16) for _ in range(2)]
        PT = [M.alloc([128, 4, 512], BF16) for _ in range(2)]
        hTe = M.alloc([128, 8, 512], BF16)
        h2T = M.alloc([128, 8, 512], BF16)
        yes_ = [M.alloc([128, 4, 1024], BF16) for _ in range(2)]
        wn = [0]

        def load_wd(e_):
            w = wdr[0]
            dma("pool", w, w[:, :, :], wdn[e_, :, :].rearrange("p (c n) -> p c n", c=8))
            dma("sp", bdf[0], bdf[0][:, :], bdn[e_, :, :])
            act(bdb[e_ % 2], bdb[e_ % 2][:, :], bdf[0], bdf[0][:, :], AF.Copy, scale=SILU_K)

        def load_wg(e_, jj):
            w = wgr[wn[0] % 4]
            wn[0] += 1
            dma("pool", w, w[:, :, :, :], wgu[e_, jj, :, :].rearrange("p (g c f) -> p g c f", g=2, c=8))
            return w

        def gen_p(e_):
            pm, pt = Pm[e_ % 2], PT[e_ % 2]
            for i in range(16):
                ts(pm, pm[:, i, :], cst_f, iota_f, selp[:, i * 32 + e_:i * 32 + e_ + 1], None, ALU.is_equal,
                   extra_reads=[selp])

        def gen_pt(e_):
            pm, pt = Pm[e_ % 2], PT[e_ % 2]
            for g in range(4):
                pp = psum()
                for k in range(4):
                    mm(pp, pp[:, k * 128:(k + 1) * 128], pm[:, g * 4 + k, :], ident_b, True, True, [pm, cst_b])
                act(pt, pt[:, g, :], pp, pp[:, :], AF.Copy)
            pgs = psum()
            for g in range(4):
                for k in range(4):
                    i = g * 4 + k
                    col = i * 32 + e_
                    mm(pgs, pgs[:, g:g + 1], pm[:, i, :], gath[:, col:col + 1], k == 0, False, [pm, gath])
                    mm(pgs, pgs[:, g:g + 1], pm[:, i, :], gatl[:, col:col + 1], False, k == 3, [pm, gatl])
            act(gsl[e_ % 2], gsl[e_ % 2][:, 0:4], pgs, pgs[:, 0:4], AF.Copy)

        pend = [load_wg(0, 0), load_wg(0, 1), load_wg(0, 2)]
        gen_p(0)
        for e_ in range(NE):
            pm, pt = Pm[e_ % 2], PT[e_ % 2]
            ye = yes_[e_ % 2]
            load_wd(e_)
            gen_pt(e_)
            for c in range(8):
                ph = psum()
                for g in range(4):
                    for k in range(4):
                        i = g * 4 + k
                        mm(ph, ph[:, g * 128:(g + 1) * 128], h1b[i][:, c * 128:(c + 1) * 128], pm[:, i, :], k == 0, k == 3,
                           [h1b[i], pm])
                act(hTe, hTe[:, c, :], ph, ph[:, :], AF.Copy)
            for jj in range(8):
                wp = pend.pop(0)
                nx = e_ * 8 + jj + 3
                if nx < NE * 8:
                    pend.append(load_wg(nx // 8, nx % 8))
                pG, pU = psum(), psum()
                for c in range(8):
                    mm(pG, pG[:, :], wp[:, 0, c, :], hTe[:, c, :], c == 0, c == 7, [wp, hTe])
                for c in range(8):
                    mm(pU, pU[:, :], wp[:, 1, c, :], hTe[:, c, :], c == 0, c == 7, [wp, hTe])
                t_s, t_u = tb[(jj % 2) * 2], tb[(jj % 2) * 2 + 1]
                act(t_s, t_s[:, :], pG, pG[:, :], AF.Silu, bias=bgk_t[:, e_ * 16 + jj:e_ * 16 + jj + 1], scale=SILU_K,
                    extra_reads=[bgk_t])
                ts(t_u, t_u[:, :], pU, pU[:, :], bgu_t[:, e_ * 16 + 8 + jj:e_ * 16 + 9 + jj], 7.0, ALU.add, ALU.min,
                   extra_reads=[bgu_t])
                ts(t_u, t_u[:, :], t_u, t_u[:, :], -7.0, 1.0, ALU.max, ALU.add)
                stt(h2T, h2T[:, jj, :], t_s, t_s[:, :], SILU_CAP, t_u, t_u[:, :], ALU.min, ALU.mult)
            if e_ + 1 < NE:
                gen_p(e_ + 1)
            wd = wdr[0]
            bb = bdb[e_ % 2]
            for st_ in range(4):
                for hf in range(2):
                    pY = psum()
                    for jj in range(8):
                        mm(pY, pY[:, :], h2T[:, jj, st_ * 128:(st_ + 1) * 128], wd[:, jj, hf * 512:(hf + 1) * 512], jj == 0, False,
                           [h2T, wd])
                    mm(pY, pY[:, :], ones_b[0:1, :], bb[0:1, hf * 512:(hf + 1) * 512], False, True, [cst_b, bb])
                    act(ye, ye[:, st_, hf * 512:(hf + 1) * 512], pY, pY[:, :], AF.Copy,
                        scale=gsl[e_ % 2][:, st_:st_ + 1], extra_reads=[gsl[e_ % 2]])
            if e_ % 2 == 1:
                pt0, ye0 = PT[(e_ - 1) % 2], yes_[(e_ - 1) % 2]
                for i in range(16):
                    g, k = i // 4, i % 4
                    for hf in range(2):
                        pC = psum()
                        mm(pC, pC[:, :], pt0[:, g, k * 128:(k + 1) * 128], ye0[:, g, hf * 512:(hf + 1) * 512], True, False,
                           [pt0, ye0])
                        mm(pC, pC[:, :], pt[:, g, k * 128:(k + 1) * 128], ye[:, g, hf * 512:(hf + 1) * 512], False, True,
                           [pt, ye])
                        tt(acc[i], acc[i][:, hf * 512:(hf + 1) * 512], acc[i], acc[i][:, hf * 512:(hf + 1) * 512], pC, pC[:, :],
                           ALU.add)

        P.barrier()
        M.cur = markc
        vec_c = M.alloc([128, 5 * 1024], F32)
        dma("sp", vec_c, vec_c[:, :], vecs[:, 3072:8192])

        def vcc(k):
            return vec_c[:, (k - 3) * 1024:(k - 2) * 1024]
        wpg_b = M.alloc([128, 8, 1024], BF16)
        wpl_b = M.alloc([128, 2, 1024], BF16)
        pT_b = M.alloc([128, 2, S], BF16)
        hh = [M.alloc([128, 1024], F32) for _ in range(2)]
        hhT = [M.alloc([128, 8, 128], BF16) for _ in range(2)]
        hqbs = [M.alloc([128, 1024], BF16) for _ in range(2)]
        zc = M.alloc([128, 1024], F32)
        zc2 = M.alloc([128, 1024], F32)
        oo = [M.alloc([128, 1024], F32) for _ in range(2)]
        stc = M.alloc([128, 32], F32)
        stc2 = M.alloc([128, 32], F32)
        zc1 = M.alloc([128, 1024], F32)
        dma("pool", wpg_b, wpg_b[:, :, :], w_pg[:, :].rearrange("p (c n) -> p c n", c=8))
        dma("pool", wpl_b, wpl_b[:, :, :], w_pl[:, :].rearrange("p (c n) -> p c n", c=2))
        dma("pool", pT_b, pT_b[:, :, :], pT[:, :, s * S:(s + 1) * S].rearrange("c p t -> p c t"))
        def c_s1(i):
            hq, hqT, hqb = hh[i % 2], hhT[i % 2], hqbs[i % 2]
            layer_norm(hq, hq[:, :], acc[i], 1024, vcc(3), vcc(4), stc, zc1, vec_c)
            act(hqb, hqb[:, :], hq, hq[:, :], AF.Copy)
            for hf in range(2):
                pt_ = psum()
                for c4 in range(4):
                    c = hf * 4 + c4
                    mm(pt_, pt_[:, c4 * 128:(c4 + 1) * 128], hqb[:, c * 128:(c + 1) * 128], ident_b, True, True,
                       [hqb, cst_b])
                P.op("act", lambda e, pt_=pt_, hqT=hqT, hf=hf: e.activation(
                    hqT[:, hf * 4:(hf + 1) * 4, :], pt_[:, :].rearrange("p (c t) -> p c t", c=4), AF.Copy),
                    reads=[pt_], writes=[hqT])

        def c_s2_pe(i):
            hqT = hhT[i % 2]
            banks = []
            for hf in range(2):
                pS_, pP_ = psum(), psum()
                for c in range(8):
                    mm(pS_, pS_[:, :], hqT[:, c, :], wpg_b[:, c, hf * 512:(hf + 1) * 512], c == 0, c == 7, [hqT, wpg_b])
                for c in range(2):
                    mm(pP_, pP_[:, :], pT_b[:, c, i * 128:(i + 1) * 128], wpl_b[:, c, hf * 512:(hf + 1) * 512], c == 0, c == 1,
                       [pT_b, wpl_b])
                banks.append((pS_, pP_))
            return banks

        def c_s2_rest(i, banks):
            ti = s * 16 + i
            hq, o_ = hh[i % 2], oo[i % 2]
            for hf in range(2):
                pS_, pP_ = banks[hf]
                tt(zc, zc[:, hf * 512:(hf + 1) * 512], pS_, pS_[:, :], vec_c, vcc(5)[:, hf * 512:(hf + 1) * 512], ALU.add)
                act(zc, zc[:, hf * 512:(hf + 1) * 512], zc, zc[:, hf * 512:(hf + 1) * 512], AF.Sigmoid)
                tt(zc, zc[:, hf * 512:(hf + 1) * 512], zc, zc[:, hf * 512:(hf + 1) * 512], pP_, pP_[:, :], ALU.mult)
            stt(zc2, zc2[:, :], hq, hq[:, :], ALPHA, zc, zc[:, :], ALU.mult, ALU.add)
            layer_norm(o_, o_[:, :], zc2, 1024, vcc(6), vcc(7), stc2, zc, vec_c)
            outT = Tl(out)
            P.stores.append(dma("sp", outT, out[ti * 128:(ti + 1) * 128, :], o_[:, :], src=o_, sem=o_))

        c_s1(0)
        for i in range(16):
            bk = c_s2_pe(i)
            if i + 1 < 16:
                c_s1(i + 1)
            c_s2_rest(i, bk)

    P.emit()
    return nc


def _consts():
    c = np.zeros((128, 768), np.float32)
    c[:, 0:128] = np.eye(128, dtype=np.float32)
    c[:, 128:256] = np.triu(np.ones((128, 128), np.float32))
    c[127, 256:384] = 1.0
    c[:, 384:512] = 1.0
    c[:, 512:640] = np.triu(np.ones((128, 128), np.float32), k=1)
    c[:, 640:768] = np.arange(128, dtype=np.float32)[None, :]
    return c


def make_in_maps(inp, cores):
    f = lambda a: np.ascontiguousarray(a, dtype=np.float32)
    w_in = inp["w_in"][0]
    b_in = inp["b_in"][0]
    cq, ck, cv, cf, cu, cgv, cga, cgb = (np.arange(0, 512), np.arange(512, 1024), np.arange(1024, 1536),
                                         np.arange(1536, 1544), np.arange(1544, 2056), np.arange(2056, 2568),
                                         np.arange(2568, 3592), np.arange(3592, 4616))
    fm = np.concatenate([cq, ck, cu, cga, cgb])
    tm = np.concatenate([cv, cgv, cf])
    rep = lambda v: f(np.broadcast_to(v[None, :], (128, v.shape[0])))
    shared = {
        "w_fm": f(w_in[:, fm].reshape(8, 128, 3584)),
        "w_tm": f(w_in[:, tm].reshape(8, 128, 1032)),
        "b_fm": f(b_in[fm].reshape(28, 128).T),
        "b_tm": rep(b_in[tm]),
        "lng": rep(inp["gmlp_ln_g"][0]),
        "lnb": rep(inp["gmlp_ln_b"][0]),
        "wsT": f(inp["w_spatial"][0].transpose(2, 0, 1).reshape(128, 1024)),
        "bsp": f(inp["b_spatial"][0].reshape(1, 1024)),
        "w_a": f(inp["w_branch_a"][0].reshape(8, 64, 1024).transpose(1, 0, 2).reshape(64, 8192)),
        "w_b": f(inp["w_branch_b"][0].reshape(4, 128, 1024).transpose(1, 0, 2).reshape(128, 4096)),
        "w_o": f(inp["w_out"][0].reshape(8, 128, 1024).transpose(1, 0, 2).reshape(128, 8192)),
        "vecs": f(np.concatenate([rep(inp[k][0]) for k in
                                  ("b_out", "ln1_g", "ln1_b", "ln2_g", "ln2_b", "b_ple_gate", "ln3_g", "ln3_b")], axis=1)),
        "w_r": f(inp["w_router"][0].reshape(8, 128, 32).transpose(1, 0, 2).reshape(128, 256)),
        "b_r": rep(inp["b_router"][0]),
        "wgu": f(inp["w_gate_up"][0].reshape(NE, 8, 128, 2, 8, 128).transpose(0, 4, 2, 3, 1, 5).reshape(NE, 8, 128, 2048)),
        "bgu": f(inp["b_gate_up"][0].reshape(NE, 16, 128).transpose(2, 0, 1).reshape(128, NE * 16)),
        "wdn": f(inp["w_down"][0].reshape(NE, 8, 128, 1024).transpose(0, 2, 1, 3).reshape(NE, 128, 8192)),
        "bdn": f(inp["b_down"][0].reshape(NE, 1, 1024)),
        "w_pl": f(inp["w_ple"][0].reshape(2, 128, 1024).transpose(1, 0, 2).reshape(128, 2048)),
        "w_pg": f(inp["w_ple_gate"][0].reshape(8, 128, 1024).transpose(1, 0, 2).reshape(128, 8192)),
        "cst": _consts(),
    }
    maps = []
    for c in cores:
        xc = inp["x"][2 * c:2 * c + 2].reshape(TOK, D)
        pc = inp["p"][0, 2 * c:2 * c + 2].reshape(TOK, 256)
        m = dict(shared)
        m["x_tm"] = f(xc)
        m["xT"] = f(xc.T.reshape(8, 128, TOK))
        m["pT"] = f(pc.T.reshape(2, 128, TOK))
        maps.append(m)
    return maps


def kernel(**inputs):
    inp = {k: np.asarray(v) for k, v in inputs.items()}
    nc = build()
    maps = make_in_maps(inp, list(range(NCORES)))
    res = run_bass_kernel_spmd(nc, maps, core_ids=list(range(NCORES)))
    outs = [np.asarray(r["out"]).reshape(2, S, D) for r in res.results]
    return np.concatenate(outs, axis=0).astype(np.float32)
```

```python
import numpy as np
import concourse.bass as bass
import concourse.mybir as mybir
from concourse.bass_utils import run_bass_kernel_spmd

F32 = mybir.dt.float32
BF16 = mybir.dt.bfloat16
AF = mybir.ActivationFunctionType
ALU = mybir.AluOpType
AX = mybir.AxisListType

NCORES = 8
D = 1024
S = 2048
NSEQ = 2
TOK = NSEQ * S
H = 8
NE = 32
ALPHA = float(2.0 ** 0.25)
EPS = 1e-5
SILU_K = 1.702
SILU_CAP = float(7.0 * SILU_K / (1.0 + np.exp(-7.0 * SILU_K)))
GELU_C = 1.5957691216057308


class St:
    __slots__ = ("w", "r", "sem", "semval")

    def __init__(self):
        self.w = None
        self.r = []
        self.sem = None
        self.semval = 0


class Tl:
    def __init__(self, h, st=None):
        self.h = h
        self.st = st if st is not None else St()

    def __getitem__(self, k):
        return self.h[k]


class Sub(Tl):
    def __init__(self, base, off, width):
        self.h, self.st, self.off, self.w = base.h, base.st, off, width

    def __getitem__(self, k):
        rows, cols = k
        a = (cols.start or 0) + self.off
        b = (cols.stop if cols.stop is not None else self.w) + self.off
        return self.h[rows, a:b]


class Op:
    __slots__ = ("eng", "fn", "deps", "inc", "val", "dma", "sem")


class Prog:
    ENG = ("pe", "act", "dve", "pool", "sp")

    def __init__(self, nc):
        self.nc = nc
        self.ops = []
        self.pending = {}
        self.last = {}
        self.dmas_since = []
        self.stores = []
        self.nsem = 0

    def op(self, eng, fn, reads=(), writes=(), dma=None, extra=()):
        o = Op()
        o.eng, o.fn, o.inc, o.val, o.dma, o.sem = eng, fn, False, 0, dma, None
        deps = set(extra)
        war = set()
        for t in reads:
            if t.st.w is not None:
                deps.add(t.st.w)
        for t in writes:
            if t.st.w is not None:
                deps.add(t.st.w)
            for rr in t.st.r:
                war.add(rr)
        for t in reads:
            if dma is None:
                t.st.r = [x for x in t.st.r if x.dma is not None or x.eng != eng]
            t.st.r.append(o)
        for t in writes:
            t.st.w = o
            t.st.r = []
        for d in war:
            deps.add(d)
        if eng == "pe" and dma is None:
            deps = {d for d in deps if d.dma is not None or d.eng != "pe"}
        if eng in self.pending:
            deps |= self.pending.pop(eng)
        deps.discard(o)
        o.deps = deps
        if dma is not None:
            st = dma.st
            if st.sem is None:
                st.sem = self.nc.alloc_semaphore("dsem%d" % self.nsem)
                self.nsem += 1
            st.semval += 16
            o.sem, o.val = st.sem, st.semval
            self.dmas_since.append(o)
        self.last[eng] = o
        self.ops.append(o)
        return o

    def barrier(self):
        deps = set(self.last.values()) | set(self.dmas_since)
        self.dmas_since = []
        for e in self.ENG:
            self.pending[e] = set(deps)

    def emit(self):
        nc = self.nc
        esem = {e: nc.alloc_semaphore("esem_" + e) for e in self.ENG}
        fin = self.op("sp", None, extra=list(self.stores))
        for o in self.ops:
            for d in o.deps:
                d.inc = True
        cnt = {e: 0 for e in self.ENG}
        for o in self.ops:
            if o.dma is None and o.inc:
                cnt[o.eng] += 1
                o.val = cnt[o.eng]
                o.sem = esem[o.eng]
        byeng = {e: [o for o in self.ops if o.eng == e] for e in self.ENG}

        def run(eng_name, eng):
            waited = {}
            for o in byeng[eng_name]:
                need = {}
                for d in o.deps:
                    k = id(d.sem)
                    if k not in need or need[k][1] < d.val:
                        need[k] = (d.sem, d.val)
                for k, (sem, val) in need.items():
                    if waited.get(k, 0) < val:
                        eng.wait_ge(sem, val)
                        waited[k] = val
                if o.fn is None:
                    continue
                ins = o.fn(eng)
                if o.dma is not None:
                    ins.then_inc(o.sem, 16)
                elif o.inc:
                    ins.then_inc(o.sem, 1)

        with nc.Block() as block:
            @block.tensor
            def _(e):
                run("pe", e)

            @block.scalar
            def _(e):
                run("act", e)

            @block.vector
            def _(e):
                run("dve", e)

            @block.gpsimd
            def _(e):
                run("pool", e)

            @block.sync
            def _(e):
                run("sp", e)


class Mem:
    BASE = 16512
    TOP = 229344

    def __init__(self, nc):
        self.nc = nc
        self.cur = self.BASE
        self.n = 0

    def alloc(self, shape, dtype, st=None):
        nb = 1
        for s_ in shape[1:]:
            nb *= s_
        nb *= 2 if dtype == BF16 else 4
        nb = (nb + 31) // 32 * 32
        assert self.cur + nb <= self.TOP, ("SBUF overflow", self.cur, nb)
        h = self.nc.alloc_sbuf_tensor_at("sb%d" % self.n, list(shape), dtype, offset=self.cur)
        self.n += 1
        self.cur += nb
        return Tl(h, st)


def build(debug=False):
    nc = bass.Bass("TRN2", target_bir_lowering=False)
    P = Prog(nc)
    M = Mem(nc)

    def din(name, shape):
        return nc.dram_tensor(name, list(shape), F32, kind="ExternalInput").ap()

    x_tm = din("x_tm", [TOK, D])
    xT = din("xT", [8, 128, TOK])
    pT = din("pT", [2, 128, TOK])
    w_fm = din("w_fm", [8, 128, 3584])
    w_tm = din("w_tm", [8, 128, 1032])
    b_fm = din("b_fm", [128, 28])
    b_tm = din("b_tm", [128, 1032])
    lng = din("lng", [128, 512])
    lnb = din("lnb", [128, 512])
    wsT = din("wsT", [128, 8 * 128])
    bsp = din("bsp", [1, 8 * 128])
    w_a = din("w_a", [64, 8 * 1024])
    w_b = din("w_b", [128, 4 * 1024])
    w_o = din("w_o", [128, 8 * 1024])
    vecs = din("vecs", [128, 8 * 1024])
    w_r = din("w_r", [128, 8 * 32])
    b_r = din("b_r", [128, 32])
    wgu = din("wgu", [NE, 8, 128, 2048])
    bgu = din("bgu", [128, NE * 16])
    wdn = din("wdn", [NE, 128, 8192])
    bdn = din("bdn", [NE, 1, 1024])
    w_pl = din("w_pl", [128, 2 * 1024])
    w_pg = din("w_pg", [128, 8 * 1024])
    cst = din("cst", [128, 6 * 128])
    out = nc.dram_tensor("out", [TOK, D], F32, kind="ExternalOutput").ap()
    h1s_h = nc.dram_tensor("h1s", [TOK, D], F32, kind="ExternalOutput").ap()
    h1s = [Tl(h1s_h) for _ in range(TOK // 128)]

    ps = [Tl(nc.alloc_psum_tensor("ps%d" % i, [128, 512], F32)) for i in range(8)]
    psn = [0]

    def psum():
        psn[0] = (psn[0] + 1) % 6
        return ps[psn[0]]
    pan = [0]

    def psum_acc():
        pan[0] += 1
        return ps[6 + pan[0] % 2]

    def dma(q, dst, dst_ap, src_ap, src=None, sem=None):
        reads = [src] if src is not None else []
        o = P.op(q, lambda e: e.dma_start(out=dst_ap, in_=src_ap), reads=reads, writes=[dst],
                 dma=(sem if sem is not None else dst))
        return o

    def mm(pst, out_ap, lhsT, rhs, start, stop, reads):
        P.op("pe", lambda e: e.matmul(out_ap, lhsT, rhs, start=start, stop=stop, skip_group_check=True),
             reads=reads, writes=[pst])

    def act(out_t, out_ap, in_t, in_ap, func, bias=0.0, scale=1.0, extra_reads=()):
        P.op("act", lambda e: e.activation(out_ap, in_ap, func, bias=bias, scale=scale),
             reads=[in_t] + list(extra_reads), writes=[out_t])

    def ts(out_t, out_ap, in_t, in_ap, s1, s2, op0, op1=None, extra_reads=()):
        if op1 is None:
            P.op("dve", lambda e: e.tensor_scalar(out_ap, in_ap, s1, None, op0),
                 reads=[in_t] + list(extra_reads), writes=[out_t])
        else:
            P.op("dve", lambda e: e.tensor_scalar(out_ap, in_ap, s1, s2, op0, op1),
                 reads=[in_t] + list(extra_reads), writes=[out_t])

    def tt(out_t, out_ap, a_t, a_ap, b_t, b_ap, op):
        P.op("dve", lambda e: e.tensor_tensor(out_ap, a_ap, b_ap, op), reads=[a_t, b_t], writes=[out_t])

    def stt(out_t, out_ap, a_t, a_ap, scalar, b_t, b_ap, op0, op1, extra_reads=()):
        P.op("dve", lambda e: e.scalar_tensor_tensor(out_ap, a_ap, scalar, b_ap, op0, op1),
             reads=[a_t, b_t] + list(extra_reads), writes=[out_t])

    cst_f = M.alloc([128, 768], F32)
    cst_b = M.alloc([128, 768], BF16)
    dma("sp", cst_f, cst_f[:, :], cst[:, :])
    dma("pool", cst_b, cst_b[:, :], cst[:, :])
    ident_f = cst_f[:, 0:128]
    triu_f = cst_f[:, 128:256]
    last_f = cst_f[:, 256:384]
    triu_b = cst_b[:, 128:256]
    ones_b = cst_b[:, 384:512]
    ident_b = cst_b[:, 0:128]
    allones_b = cst_b[:, 384:512]
    sut_b = cst_b[:, 512:640]
    iota_f = cst_f[:, 640:768]
    mark0 = M.cur
    vec_t = M.alloc([128, 3 * 1024], F32)
    dma("sp", vec_t, vec_t[:, :], vecs[:, 0:3072])

    def vec(k):
        return vec_t[:, k * 1024:(k + 1) * 1024]

    def gelu(dst_t, dst_ap, src_t, src_ap, tmp_t, tmp_ap):
        tt(tmp_t, tmp_ap, src_t, src_ap, src_t, src_ap, ALU.mult)
        ts(tmp_t, tmp_ap, tmp_t, tmp_ap, 0.044715, 1.0, ALU.mult, ALU.add)
        tt(tmp_t, tmp_ap, tmp_t, tmp_ap, src_t, src_ap, ALU.mult)
        act(tmp_t, tmp_ap, tmp_t, tmp_ap, AF.Sigmoid, scale=GELU_C)
        tt(dst_t, dst_ap, tmp_t, tmp_ap, src_t, src_ap, ALU.mult)

    def layer_norm(dst_t, dst_ap, src_t, W, g_ap, b_ap, st_t, tmp_t, gb_t):
        nch = W // 512
        for ch in range(nch):
            P.op("dve", lambda e, ch=ch: e.bn_stats(st_t[:, 6 * ch:6 * ch + 6], src_t[:, 512 * ch:512 * ch + 512]),
                 reads=[src_t], writes=[st_t])
        P.op("dve", lambda e: e.bn_aggr(st_t[:, 16:18], st_t[:, 0:6 * nch]), reads=[st_t], writes=[st_t])
        act(st_t, st_t[:, 19:20], st_t, st_t[:, 17:18], AF.Ln, bias=EPS)
        act(st_t, st_t[:, 18:19], st_t, st_t[:, 19:20], AF.Exp, scale=-0.5)
        if W >= 1024:
            ts(st_t, st_t[:, 20:21], st_t, st_t[:, 16:17], st_t[:, 18:19], -1.0, ALU.mult, ALU.mult)
            act(tmp_t, tmp_t[:, 0:W], src_t, src_t[:, 0:W], AF.Identity, bias=st_t[:, 20:21], scale=st_t[:, 18:19],
                extra_reads=[st_t])
        else:
            ts(tmp_t, tmp_t[:, 0:W], src_t, src_t[:, 0:W], st_t[:, 16:17], st_t[:, 18:19], ALU.subtract, ALU.mult,
               extra_reads=[st_t])
        tt(tmp_t, tmp_t[:, 0:W], tmp_t, tmp_t[:, 0:W], gb_t, g_ap, ALU.mult)
        tt(dst_t, dst_ap, tmp_t, tmp_t[:, 0:W], gb_t, b_ap, ALU.add)

    RS = 6
    ring = [M.alloc([128, 4096], BF16) for _ in range(RS)]
    rn = [0]

    def wload(src_ap, view, npart=128):
        sl = ring[rn[0] % RS]
        rn[0] += 1
        dma("pool", sl, view(sl), src_ap)
        return sl

    kT = M.alloc([128, 4, S], BF16)
    vaug = M.alloc([128, 16, H * 65], BF16)
    cneg = M.alloc([128, 16 * 8], F32)
    lsb = M.alloc([128, 16 * 16], BF16)
    rcb = [M.alloc([128, 1024], BF16) for _ in range(2)]
    biasq = M.alloc([128, 16 * 8], F32)
    wsT_b = M.alloc([128, 8 * 128], BF16)
    bsp_b = M.alloc([1, 8 * 128], BF16)
    btm_t = M.alloc([128, 1032], F32)
    bfm_t = M.alloc([128, 28], F32)
    lngb = M.alloc([128, 1024], F32)
    XT = [M.alloc([128, 8, 512], BF16) for _ in range(2)]
    qT = M.alloc([128, 4, 512], BF16)
    uT = M.alloc([128, 4, 512], BF16)
    attnT = M.alloc([128, 8, 512], BF16)
    sguT = M.alloc([128, 4, 512], BF16)
    vn = M.alloc([128, 4, 512], BF16)
    mrgT = M.alloc([128, 8, 512], BF16)
    ta = [M.alloc([128, 512], F32) for _ in range(4)]
    ex = [M.alloc([128, 512], BF16) for _ in range(4)]
    exn = [0]
    smf = [M.alloc([128, 24], F32) for _ in range(4)]
    stq = [M.alloc([128, 32], F32) for _ in range(4)]
    rcp = [M.alloc([128, 512], F32) for _ in range(2)]
    otm = rcp
    xt = [M.alloc([128, 1024], F32) for _ in range(2)]
    zt = M.alloc([128, 1024], F32)
    zt2 = M.alloc([128, 1024], F32)
    zg = [Sub(zt, 0, 512), Sub(zt, 512, 512), Sub(zt2, 0, 512), Sub(zt2, 512, 512)]
    h1o = [M.alloc([128, 1024], F32) for _ in range(2)]
    stt_t = M.alloc([128, 32], F32)
    sm = M.alloc([128, 64], F32)

    bo_b = M.alloc([1, 2048], BF16)
    ts(bo_b, bo_b[0:1, 0:1024], vec_t, vec_t[0:1, 0:1024], 1.0, None, ALU.mult)
    tt(bo_b, bo_b[0:1, 1024:2048], vec_t, vec_t[0:1, 0:1024], bo_b, bo_b[0:1, 0:1024], ALU.subtract)
    dma("pool", wsT_b, wsT_b[:, :], wsT[:, :])
    dma("pool", bsp_b, bsp_b[:, :], bsp[:, :])
    dma("sp", btm_t, btm_t[:, :], b_tm[:, :])
    dma("sp", bfm_t, bfm_t[:, :], b_fm[:, :])
    dma("sp", lngb, lngb[:, 0:512], lng[:, :])
    dma("sp", lngb, lngb[:, 512:1024], lnb[:, :])
    for g in range(8):
        tt(wsT_b, wsT_b[:, g * 128:(g + 1) * 128], wsT_b, wsT_b[:, g * 128:(g + 1) * 128], cst_b, triu_b, ALU.mult)
    P.op("dve", lambda e: e.memset(vaug[:, :, :], 1.0), writes=[vaug])

    def v3(sl, c):
        return sl[:, :].rearrange("p (c n) -> p c n", c=c)

    xtn = [0]
    for s in range(NSEQ):
        for j in range(4):
            T0 = s * S + j * 512
            X = XT[xtn[0] % 2]
            xtn[0] += 1
            dma("pool", X, X[:, :, :], xT[:, :, T0:T0 + 512].rearrange("c p t -> p c t"))
            wv = wload(w_tm[:, :, 0:512].rearrange("c p n -> p c n"), lambda sl: v3(sl, 8))
            wg = wload(w_tm[:, :, 512:1024].rearrange("c p n -> p c n"), lambda sl: v3(sl, 8))
            wf = wload(w_tm[:, :, 1024:1032].rearrange("c p n -> p c n"), lambda sl: sl[:, 0:64].rearrange("p (c n) -> p c n", c=8))
            for i in range(4):
                ti = j * 4 + i
                pv, pg, pf = psum(), psum(), psum()
                for c in range(8):
                    lw = X[:, c, i * 128:(i + 1) * 128]
                    mm(pv, pv[:, :], lw, v3(wv, 8)[:, c, :], c == 0, c == 7, [X, wv])
                for c in range(8):
                    lw = X[:, c, i * 128:(i + 1) * 128]
                    mm(pg, pg[:, :], lw, v3(wg, 8)[:, c, :], c == 0, c == 7, [X, wg])
                for c in range(8):
                    lw = X[:, c, i * 128:(i + 1) * 128]
                    mm(pf, pf[:, 0:8], lw, wf[:, 0:64].rearrange("p (c n) -> p c n", c=8)[:, c, :], c == 0, c == 7, [X, wf])
                P.op("dve", lambda e, pv=pv, ti=ti: e.tensor_tensor(
                    vaug[:, ti, :].rearrange("p (h d) -> p h d", h=H)[:, :, 0:64],
                    pv[:, :].rearrange("p (h d) -> p h d", h=H),
                    btm_t[:, 0:512].rearrange("p (h d) -> p h d", h=H), ALU.add),
                    reads=[pv, btm_t], writes=[vaug])
                tt(zg[i], zg[i][:, :], pg, pg[:, :], btm_t, btm_t[:, 512:1024], ALU.add)
                tt(smf[i], smf[i][:, 0:8], pf, pf[:, 0:8], btm_t, btm_t[:, 1024:1032], ALU.add)
                act(smf[i], smf[i][:, 8:16], smf[i], smf[i][:, 0:8], AF.Exp, scale=-1.0)
                act(smf[i], smf[i][:, 16:24], smf[i], smf[i][:, 8:16], AF.Ln, bias=1.0)

            def tm_tail(j=j):
                for i in range(4):
                    ti = j * 4 + i
                    ts(lsb, lsb[:, ti * 16:ti * 16 + 8], smf[i], smf[i][:, 16:24], 1.0, None, ALU.mult)
                    tt(lsb, lsb[:, ti * 16 + 8:ti * 16 + 16], smf[i], smf[i][:, 16:24], lsb, lsb[:, ti * 16:ti * 16 + 8],
                       ALU.subtract)
                for i in range(4):
                    ti = j * 4 + i
                    pc = psum()
                    for t2 in range(ti):
                        mm(pc, pc[:, 0:8], allones_b, lsb[:, t2 * 16:t2 * 16 + 8], t2 == 0, False, [cst_b, lsb])
                        mm(pc, pc[:, 0:8], allones_b, lsb[:, t2 * 16 + 8:t2 * 16 + 16], False, False, [cst_b, lsb])
                    mm(pc, pc[:, 0:8], triu_b, lsb[:, ti * 16:ti * 16 + 8], ti == 0, False, [cst_b, lsb])
                    mm(pc, pc[:, 0:8], triu_b, lsb[:, ti * 16 + 8:ti * 16 + 16], False, True, [cst_b, lsb])
                    P.op("act", lambda e, pc=pc, ti=ti: e.activation(cneg[:, ti * 8:(ti + 1) * 8], pc[:, 0:8], AF.Copy),
                         reads=[pc], writes=[cneg])

            def tm_gv():
                for i in range(4):
                    gelu(zg[i], zg[i][:, :], zg[i], zg[i][:, :], ta[i % 2], ta[i % 2][:, :])
                for i in range(4):
                    layer_norm(vn, vn[:, i, :], zg[i], 512, lngb[:, 0:512], lngb[:, 512:1024], stq[i], ta[2 + i % 2], lngb)
            def tm_bias(j=j):
              tref = j * 4 + 3
              pr = psum()
              for t2 in range(tref + 1):
                  mm(pr, pr[:, 0:8], allones_b, lsb[:, t2 * 16:t2 * 16 + 8], t2 == 0, False, [cst_b, lsb])
                  mm(pr, pr[:, 0:8], allones_b, lsb[:, t2 * 16 + 8:t2 * 16 + 16], False, t2 == tref, [cst_b, lsb])
              act(sm, sm[:, 24:32], pr, pr[:, 0:8], AF.Copy)
              for ti in range(tref + 1):
                tt(biasq, biasq[:, ti * 8:(ti + 1) * 8], cneg, cneg[:, ti * 8:(ti + 1) * 8], sm, sm[:, 24:32], ALU.subtract)
            tref_unused = j * 4 + 3
            for blk in range(3):
                wp = wload(w_fm[:, :, blk * 512:(blk + 1) * 512].rearrange("c p n -> p c n"), lambda sl: v3(sl, 8))
                for m in range(4):
                    pq = psum()
                    for c in range(8):
                        mm(pq, pq[:, :], v3(wp, 8)[:, c, m * 128:(m + 1) * 128], X[:, c, :], c == 0, c == 7, [wp, X])
                    bcol = bfm_t[:, blk * 4 + m:blk * 4 + m + 1]
                    if blk == 0:
                        act(qT, qT[:, m, :], pq, pq[:, :], AF.Identity, bias=bcol, extra_reads=[bfm_t])
                    elif blk == 1:
                        act(kT, kT[:, m, j * 512:(j + 1) * 512], pq, pq[:, :], AF.Identity, bias=bcol, extra_reads=[bfm_t])
                    else:
                        t0_ = ta[2 + m % 2]
                        act(t0_, t0_[:, :], pq, pq[:, :], AF.Identity, bias=bcol, extra_reads=[bfm_t])
                        gelu(uT, uT[:, m, :], t0_, t0_[:, :], ta[m % 2], ta[m % 2][:, :])
                if blk == 0:
                    tm_tail()
                    tm_bias()
                if blk == 1:
                    tm_gv()
            pend_tail = []
            for h in range(H):
                pcn, pb = h // 2, (h % 2) * 64
                po = psum_acc()
                nkt = 4 * j + 4
                live = {}

                def issue_s(ki, pcn=pcn, pb=pb):
                    dj = ki - 4 * j
                    q0 = 0 if dj < 0 else dj * 128
                    pS = psum()
                    mm(pS, pS[:, q0:512], kT[pb:pb + 64, pcn, ki * 128:(ki + 1) * 128], qT[pb:pb + 64, pcn, q0:512],
                       True, True, [kT, qT])
                    live[ki] = (pS, q0, dj)
                issue_s(0)
                if nkt > 1:
                    issue_s(1)
                if pend_tail:
                    pend_tail.pop(0)()
                for ki in range(nkt):
                    pS, q0, dj = live.pop(ki)
                    E = ex[exn[0] % 4]
                    exn[0] += 1
                    act(E, E[:, q0:512], pS, pS[:, q0:512], AF.Exp, bias=biasq[:, ki * 8 + h:ki * 8 + h + 1],
                        scale=0.125, extra_reads=[biasq])
                    if dj >= 0:
                        tt(E, E[:, q0:q0 + 128], E, E[:, q0:q0 + 128], cst_b, triu_b, ALU.mult)
                    if ki + 2 < nkt:
                        issue_s(ki + 2)
                    mm(po, po[0:65, q0:512], vaug[:, ki, h * 65:(h + 1) * 65], E[:, q0:512], ki == 0, ki == nkt - 1,
                       [vaug, E])

                def tail(po=po, h=h):
                    rc, ot = rcp[h % 2], otm[h % 2]
                    act(rc, rc[64:65, :], po, po[64:65, :], AF.Ln)
                    act(rc, rc[64:65, :], rc, rc[64:65, :], AF.Exp, scale=-1.0)
                    rb = rcb[h % 2]
                    ts(rb, rb[64:65, 0:512], rc, rc[64:65, :], 1.0, None, ALU.mult)
                    tt(rb, rb[64:65, 512:1024], rc, rc[64:65, :], rb, rb[64:65, 0:512], ALU.subtract)
                    pbc = psum()
                    mm(pbc, pbc[0:64, :], cst_b[64:65, 384:448], rb[64:65, 0:512], True, False, [cst_b, rb])
                    mm(pbc, pbc[0:64, :], cst_b[64:65, 384:448], rb[64:65, 512:1024], False, True, [cst_b, rb])
                    act(ot, ot[0:64, :], po, po[0:64, :], AF.Copy)
                    tt(attnT, attnT[0:64, h, :], ot, ot[0:64, :], pbc, pbc[0:64, :], ALU.mult)
                pend_tail.append(tail)
            pend_tail.pop(0)()
            for g in range(8):
                pcn, pb = g // 2, (g % 2) * 64
                pg_ = psum()
                for i in range(4):
                    mm(pg_, pg_[:, i * 128:(i + 1) * 128], vn[:, i, pcn * 128:(pcn + 1) * 128],
                       wsT_b[:, g * 128:(g + 1) * 128], True, False, [vn, wsT_b])
                    mm(pg_, pg_[:, i * 128:(i + 1) * 128], ones_b[0:1, :], bsp_b[0:1, g * 128:(g + 1) * 128],
                       False, True, [cst_b, bsp_b])
                tt(sguT, sguT[pb:pb + 64, pcn, :], uT, uT[pb:pb + 64, pcn, :], pg_, pg_[pb:pb + 64, :], ALU.mult)
            wa0 = wload(w_a[:, :].rearrange("p (h n) -> p h n", h=8)[:, :, 0:512], lambda sl: sl[0:64, :].rearrange("p (h n) -> p h n", h=8))
            wa1 = wload(w_a[:, :].rearrange("p (h n) -> p h n", h=8)[:, :, 512:1024], lambda sl: sl[0:64, :].rearrange("p (h n) -> p h n", h=8))
            wb_ = wload(w_b[:, :], lambda sl: sl[:, :])
            for mb in range(2):
                wga = wload(w_fm[:, :, 1536 + mb * 512:1536 + (mb + 1) * 512].rearrange("c p n -> p c n"), lambda sl: v3(sl, 8))
                wgb = wload(w_fm[:, :, 2560 + mb * 512:2560 + (mb + 1) * 512].rearrange("c p n -> p c n"), lambda sl: v3(sl, 8))
                wa = wa0 if mb == 0 else wa1
                for mi in range(4):
                    m = mb * 4 + mi
                    pGA, pGB, pA, pB = psum(), psum(), psum(), psum()
                    for c in range(8):
                        mm(pGA, pGA[:, :], v3(wga, 8)[:, c, mi * 128:(mi + 1) * 128], X[:, c, :], c == 0, c == 7, [wga, X])
                    for c in range(8):
                        mm(pGB, pGB[:, :], v3(wgb, 8)[:, c, mi * 128:(mi + 1) * 128], X[:, c, :], c == 0, c == 7, [wgb, X])
                    for h in range(8):
                        mm(pA, pA[:, :], wa[0:64, :].rearrange("p (h n) -> p h n", h=8)[:, h, mi * 128:(mi + 1) * 128],
                           attnT[0:64, h, :], h == 0, h == 7, [wa, attnT])
                    for c in range(4):
                        mm(pB, pB[:, :], wb_[:, c * 1024 + m * 128:c * 1024 + (m + 1) * 128], sguT[:, c, :], c == 0, c == 3,
                           [wb_, sguT])
                    g0_, g1_ = ta[(m % 2) * 2], ta[(m % 2) * 2 + 1]
                    act(g0_, g0_[:, :], pGA, pGA[:, :], AF.Sigmoid, bias=bfm_t[:, 12 + m:13 + m], extra_reads=[bfm_t])
                    act(g1_, g1_[:, :], pGB, pGB[:, :], AF.Sigmoid, bias=bfm_t[:, 20 + m:21 + m], extra_reads=[bfm_t])
                    tt(g0_, g0_[:, :], g0_, g0_[:, :], pA, pA[:, :], ALU.mult)
                    tt(g1_, g1_[:, :], g1_, g1_[:, :], pB, pB[:, :], ALU.mult)
                    tt(mrgT, mrgT[:, m, :], g0_, g0_[:, :], g1_, g1_[:, :], ALU.add)
            wo0 = wload(w_o[:, 0:4096], lambda sl: sl[:, :])
            wo1 = wload(w_o[:, 4096:8192], lambda sl: sl[:, :])
            for i in range(4):
                ti = (T0 // 128) + i
                xx = xt[ti % 2]
                dma("sp", xx, xx[:, :], x_tm[ti * 128:(ti + 1) * 128, :])
                for hf in range(2):
                    pz = psum()
                    for c in range(8):
                        wo = wo0 if c < 4 else wo1
                        cc = c % 4
                        mm(pz, pz[:, :], mrgT[:, c, i * 128:(i + 1) * 128],
                           wo[:, cc * 1024 + hf * 512:cc * 1024 + (hf + 1) * 512], c == 0, False, [mrgT, wo])
                    mm(pz, pz[:, :], ones_b[0:1, :], bo_b[0:1, hf * 512:(hf + 1) * 512], False, False, [cst_b, bo_b])
                    mm(pz, pz[:, :], ones_b[0:1, :], bo_b[0:1, 1024 + hf * 512:1024 + (hf + 1) * 512], False, True, [cst_b, bo_b])
                    stt(zt2, zt2[:, hf * 512:(hf + 1) * 512], xx, xx[:, hf * 512:(hf + 1) * 512], ALPHA, pz, pz[:, :],
                        ALU.mult, ALU.add)
                ho = h1o[ti % 2]
                layer_norm(ho, ho[:, :], zt2, 1024, vec(1), vec(2), stt_t, zt, vec_t)
                dma("sp", h1s[ti], h1s_h[ti * 128:(ti + 1) * 128, :], ho[:, :], src=ho, sem=ho)

    for s in range(NSEQ):
        P.barrier()
        M.cur = mark0
        acc = [M.alloc([128, 1024], F32) for _ in range(16)]
        markc = M.cur
        h1b = [M.alloc([128, 1024], BF16) for _ in range(16)]
        selp = M.alloc([128, 16 * 32], F32)
        gath = M.alloc([128, 16 * 32], BF16)
        gatl = M.alloc([128, 16 * 32], BF16)
        gsl = [M.alloc([128, 4], F32) for _ in range(2)]
        wr_b = M.alloc([128, 8 * 32], BF16)
        br_t = M.alloc([128, 32], F32)
        bgu_t = M.alloc([128, NE * 16], F32)
        bgk_t = M.alloc([128, NE * 16], F32)
        wgr = [M.alloc([128, 2, 8, 128], BF16) for _ in range(4)]
        wdr = [M.alloc([128, 8, 1024], BF16) for _ in range(1)]
        bdf = [M.alloc([1, 1024], F32) for _ in range(1)]
        bdb = [M.alloc([1, 1024], BF16) for _ in range(2)]
        sm2s = [M.alloc([128, 128], F32) for _ in range(2)]
        markh = M.cur
        hst = [M.alloc([128, 1024], F32) for _ in range(2)]
        hTt = [M.alloc([128, 8, 128], BF16) for _ in range(2)]
        gat = M.alloc([128, 16 * 32], F32)
        maskb = M.alloc([128, 16 * 32], BF16)
        dma("pool", wr_b, wr_b[:, :], w_r[:, :])
        dma("sp", br_t, br_t[:, :], b_r[:, :])
        dma("sp", bgu_t, bgu_t[:, :], bgu[:, :])
        act(bgk_t, bgk_t[:, :], bgu_t, bgu_t[:, :], AF.Copy, scale=SILU_K)

        pls = {}

        def m0_a(i):
            ti = s * 16 + i
            hs = hst[i % 2]
            hT = hTt[i % 2]
            dma("sp", hs, hs[:, :], h1s_h[ti * 128:(ti + 1) * 128, :], src=h1s[ti])
            act(acc[i], acc[i][:, :], hs, hs[:, :], AF.Copy, scale=ALPHA)
            act(h1b[i], h1b[i][:, :], hs, hs[:, :], AF.Copy)
            for hf in range(2):
                pt_ = psum()
                for c4 in range(4):
                    c = hf * 4 + c4
                    mm(pt_, pt_[:, c4 * 128:(c4 + 1) * 128], h1b[i][:, c * 128:(c + 1) * 128], ident_b, True, True,
                       [h1b[i], cst_b])
                P.op("act", lambda e, pt_=pt_, hT=hT, hf=hf: e.activation(
                    hT[:, hf * 4:(hf + 1) * 4, :], pt_[:, :].rearrange("p (c t) -> p c t", c=4), AF.Copy),
                    reads=[pt_], writes=[hT])
            pl = psum()
            for c in range(8):
                mm(pl, pl[:, 0:32], hT[:, c, :], wr_b[:, c * 32:(c + 1) * 32], c == 0, c == 7, [hT, wr_b])
            pls[i] = pl

        def m0_b(i):
            pl = pls.pop(i)
            sm2 = sm2s[i % 2]
            tt(sm2, sm2[:, 0:32], pl, pl[:, 0:32], br_t, br_t[:, :], ALU.add)
            P.op("dve", lambda e: e.max(out=sm2[:, 32:40], in_=sm2[:, 0:32]), reads=[sm2], writes=[sm2])
            ts(sm2, sm2[:, 40:72], sm2, sm2[:, 0:32], sm2[:, 35:36], None, ALU.is_ge)
            ts(maskb, maskb[:, i * 32:(i + 1) * 32], sm2, sm2[:, 0:32], sm2[:, 35:36], None, ALU.is_ge)
            ts(sm2, sm2[:, 72:73], sm2, sm2[:, 32:33], -1.0, None, ALU.mult)
            act(sm2, sm2[:, 80:112], sm2, sm2[:, 0:32], AF.Exp, bias=sm2[:, 72:73])
            tt(sm2, sm2[:, 80:112], sm2, sm2[:, 80:112], sm2, sm2[:, 40:72], ALU.mult)
            P.op("dve", lambda e: e.reduce_sum(sm2[:, 73:74], sm2[:, 80:112], axis=AX.X), reads=[sm2], writes=[sm2])
            P.op("dve", lambda e: e.reciprocal(sm2[:, 74:75], sm2[:, 73:74]), reads=[sm2], writes=[sm2])
            ts(gat, gat[:, i * 32:(i + 1) * 32], sm2, sm2[:, 80:112], sm2[:, 74:75], 1.0 / SILU_K, ALU.mult, ALU.mult)
            ts(gath, gath[:, i * 32:(i + 1) * 32], gat, gat[:, i * 32:(i + 1) * 32], 1.0, None, ALU.mult)
            tt(gatl, gatl[:, i * 32:(i + 1) * 32], gat, gat[:, i * 32:(i + 1) * 32], gath, gath[:, i * 32:(i + 1) * 32],
               ALU.subtract)
            g0, k = (i // 4) * 4, i % 4
            prk = psum()
            for k2 in range(k):
                mm(prk, prk[:, 0:32], ones_b, maskb[:, (g0 + k2) * 32:(g0 + k2 + 1) * 32], k2 == 0, False, [cst_b, maskb])
            mm(prk, prk[:, 0:32], sut_b, maskb[:, i * 32:(i + 1) * 32], k == 0, True, [cst_b, maskb])
            stt(selp, selp[:, i * 32:(i + 1) * 32], prk, prk[:, 0:32], 1.0, sm2, sm2[:, 40:72], ALU.add, ALU.mult)
            ts(selp, selp[:, i * 32:(i + 1) * 32], selp, selp[:, i * 32:(i + 1) * 32], -1.0, None, ALU.add)

        m0_a(0)
        for i in range(16):
            if i + 1 < 16:
                m0_a(i + 1)
            m0_b(i)

        P.barrier()
        M.cur = markh
        tb = [M.alloc([128, 512], F32) for _ in range(4)]
        Pm = [M.alloc([128, 16, 128], BF16) for _ in range(2)]
        PT = [M.alloc([128, 4, 512], BF16) for _ in range(2)]
        hTe = [M.alloc([128, 512], BF16) for _ in range(8)]
        h2T = [M.alloc([128, 512], BF16) for _ in range(8)]
        yes_ = [[M.alloc([128, 1024], BF16) for _ in range(4)] for _ in range(2)]
        wn = [0]

        def load_wd(e_):
            w = wdr[0]
            dma("pool", w, w[:, :, :], wdn[e_, :, :].rearrange("p (c n) -> p c n", c=8))
            dma("sp", bdf[0], bdf[0][:, :], bdn[e_, :, :])
            act(bdb[e_ % 2], bdb[e_ % 2][:, :], bdf[0], bdf[0][:, :], AF.Copy, scale=SILU_K)

        def load_wg(e_, jj):
            w = wgr[wn[0] % 4]
            wn[0] += 1
            dma("pool", w, w[:, :, :, :], wgu[e_, jj, :, :].rearrange("p (g c f) -> p g c f", g=2, c=8))
            return w

        def gen_p(e_):
            pm, pt = Pm[e_ % 2], PT[e_ % 2]
            for i in range(16):
                ts(pm, pm[:, i, :], cst_f, iota_f, selp[:, i * 32 + e_:i * 32 + e_ + 1], None, ALU.is_equal,
                   extra_reads=[selp])

        def gen_pt(e_):
            pm, pt = Pm[e_ % 2], PT[e_ % 2]
            for g in range(4):
                pp = psum()
                for k in range(4):
                    mm(pp, pp[:, k * 128:(k + 1) * 128], pm[:, g * 4 + k, :], ident_b, True, True, [pm, cst_b])
                act(pt, pt[:, g, :], pp, pp[:, :], AF.Copy)
            pgs = psum()
            for g in range(4):
                for k in range(4):
                    i = g * 4 + k
                    col = i * 32 + e_
                    mm(pgs, pgs[:, g:g + 1], pm[:, i, :], gath[:, col:col + 1], k == 0, False, [pm, gath])
                    mm(pgs, pgs[:, g:g + 1], pm[:, i, :], gatl[:, col:col + 1], False, k == 3, [pm, gatl])
            act(gsl[e_ % 2], gsl[e_ % 2][:, 0:4], pgs, pgs[:, 0:4], AF.Copy)

        pend = [load_wg(0, 0), load_wg(0, 1), load_wg(0, 2)]
        gen_p(0)
        for e_ in range(NE):
            pm, pt = Pm[e_ % 2], PT[e_ % 2]
            ye = yes_[e_ % 2]
            load_wd(e_)
            gen_pt(e_)
            for c in range(8):
                ph = psum()
                for g in range(4):
                    for k in range(4):
                        i = g * 4 + k
                        mm(ph, ph[:, g * 128:(g + 1) * 128], h1b[i][:, c * 128:(c + 1) * 128], pm[:, i, :], k == 0, k == 3,
                           [h1b[i], pm])
                act(hTe[c], hTe[c][:, :], ph, ph[:, :], AF.Copy)
            for jj in range(8):
                wp = pend.pop(0)
                nx = e_ * 8 + jj + 3
                if nx < NE * 8:
                    pend.append(load_wg(nx // 8, nx % 8))
                pG, pU = psum(), psum()
                for c in range(8):
                    mm(pG, pG[:, :], wp[:, 0, c, :], hTe[c][:, :], c == 0, c == 7, [wp, hTe[c]])
                for c in range(8):
                    mm(pU, pU[:, :], wp[:, 1, c, :], hTe[c][:, :], c == 0, c == 7, [wp, hTe[c]])
                t_s, t_u = tb[(jj % 2) * 2], tb[(jj % 2) * 2 + 1]
                act(t_s, t_s[:, :], pG, pG[:, :], AF.Silu, bias=bgk_t[:, e_ * 16 + jj:e_ * 16 + jj + 1], scale=SILU_K,
                    extra_reads=[bgk_t])
                ts(t_u, t_u[:, :], pU, pU[:, :], bgu_t[:, e_ * 16 + 8 + jj:e_ * 16 + 9 + jj], 7.0, ALU.add, ALU.min,
                   extra_reads=[bgu_t])
                ts(t_u, t_u[:, :], t_u, t_u[:, :], -7.0, 1.0, ALU.max, ALU.add)
                stt(h2T[jj], h2T[jj][:, :], t_s, t_s[:, :], SILU_CAP, t_u, t_u[:, :], ALU.min, ALU.mult)
            if e_ + 1 < NE:
                gen_p(e_ + 1)
            wd = wdr[0]
            bb = bdb[e_ % 2]
            for st_ in range(4):
                for hf in range(2):
                    pY = psum()
                    for jj in range(8):
                        mm(pY, pY[:, :], h2T[jj][:, st_ * 128:(st_ + 1) * 128], wd[:, jj, hf * 512:(hf + 1) * 512], jj == 0, False,
                           [h2T[jj], wd])
                    mm(pY, pY[:, :], ones_b[0:1, :], bb[0:1, hf * 512:(hf + 1) * 512], False, True, [cst_b, bb])
                    act(ye[st_], ye[st_][:, hf * 512:(hf + 1) * 512], pY, pY[:, :], AF.Copy,
                        scale=gsl[e_ % 2][:, st_:st_ + 1], extra_reads=[gsl[e_ % 2]])
            if e_ % 2 == 1:
                pt0, ye0 = PT[(e_ - 1) % 2], yes_[(e_ - 1) % 2]
                for i in range(16):
                    g, k = i // 4, i % 4
                    for hf in range(2):
                        pC = psum()
                        mm(pC, pC[:, :], pt0[:, g, k * 128:(k + 1) * 128], ye0[g][:, hf * 512:(hf + 1) * 512], True, False,
                           [pt0, ye0[g]])
                        mm(pC, pC[:, :], pt[:, g, k * 128:(k + 1) * 128], ye[g][:, hf * 512:(hf + 1) * 512], False, True,
                           [pt, ye[g]])
                        tt(acc[i], acc[i][:, hf * 512:(hf + 1) * 512], acc[i], acc[i][:, hf * 512:(hf + 1) * 512], pC, pC[:, :],
                           ALU.add)

        P.barrier()
        M.cur = markc
        vec_c = M.alloc([128, 5 * 1024], F32)
        dma("sp", vec_c, vec_c[:, :], vecs[:, 3072:8192])

        def vcc(k):
            return vec_c[:, (k - 3) * 1024:(k - 2) * 1024]
        wpg_b = M.alloc([128, 8, 1024], BF16)
        wpl_b = M.alloc([128, 2, 1024], BF16)
        pT_b = M.alloc([128, 2, S], BF16)
        hh = [M.alloc([128, 1024], F32) for _ in range(2)]
        hhT = [M.alloc([128, 8, 128], BF16) for _ in range(2)]
        hqbs = [M.alloc([128, 1024], BF16) for _ in range(2)]
        zc = M.alloc([128, 1024], F32)
        zc2 = M.alloc([128, 1024], F32)
        oo = [M.alloc([128, 1024], F32) for _ in range(2)]
        stc = M.alloc([128, 32], F32)
        stc2 = M.alloc([128, 32], F32)
        zc1 = M.alloc([128, 1024], F32)
        dma("pool", wpg_b, wpg_b[:, :, :], w_pg[:, :].rearrange("p (c n) -> p c n", c=8))
        dma("pool", wpl_b, wpl_b[:, :, :], w_pl[:, :].rearrange("p (c n) -> p c n", c=2))
        dma("pool", pT_b, pT_b[:, :, :], pT[:, :, s * S:(s + 1) * S].rearrange("c p t -> p c t"))
        def c_s1(i):
            hq, hqT, hqb = hh[i % 2], hhT[i % 2], hqbs[i % 2]
            layer_norm(hq, hq[:, :], acc[i], 1024, vcc(3), vcc(4), stc, zc1, vec_c)
            act(hqb, hqb[:, :], hq, hq[:, :], AF.Copy)
            for hf in range(2):
                pt_ = psum()
                for c4 in range(4):
                    c = hf * 4 + c4
                    mm(pt_, pt_[:, c4 * 128:(c4 + 1) * 128], hqb[:, c * 128:(c + 1) * 128], ident_b, True, True,
                       [hqb, cst_b])
                P.op("act", lambda e, pt_=pt_, hqT=hqT, hf=hf: e.activation(
                    hqT[:, hf * 4:(hf + 1) * 4, :], pt_[:, :].rearrange("p (c t) -> p c t", c=4), AF.Copy),
                    reads=[pt_], writes=[hqT])

        def c_s2_pe(i):
            hqT = hhT[i % 2]
            banks = []
            for hf in range(2):
                pS_, pP_ = psum(), psum()
                for c in range(8):
                    mm(pS_, pS_[:, :], hqT[:, c, :], wpg_b[:, c, hf * 512:(hf + 1) * 512], c == 0, c == 7, [hqT, wpg_b])
                for c in range(2):
                    mm(pP_, pP_[:, :], pT_b[:, c, i * 128:(i + 1) * 128], wpl_b[:, c, hf * 512:(hf + 1) * 512], c == 0, c == 1,
                       [pT_b, wpl_b])
                banks.append((pS_, pP_))
            return banks

        def c_s2_rest(i, banks):
            ti = s * 16 + i
            hq, o_ = hh[i % 2], oo[i % 2]
            for hf in range(2):
                pS_, pP_ = banks[hf]
                tt(zc, zc[:, hf * 512:(hf + 1) * 512], pS_, pS_[:, :], vec_c, vcc(5)[:, hf * 512:(hf + 1) * 512], ALU.add)
                act(zc, zc[:, hf * 512:(hf + 1) * 512], zc, zc[:, hf * 512:(hf + 1) * 512], AF.Sigmoid)
                tt(zc, zc[:, hf * 512:(hf + 1) * 512], zc, zc[:, hf * 512:(hf + 1) * 512], pP_, pP_[:, :], ALU.mult)
            stt(zc2, zc2[:, :], hq, hq[:, :], ALPHA, zc, zc[:, :], ALU.mult, ALU.add)
            layer_norm(o_, o_[:, :], zc2, 1024, vcc(6), vcc(7), stc2, zc, vec_c)
            outT = Tl(out)
            P.stores.append(dma("sp", outT, out[ti * 128:(ti + 1) * 128, :], o_[:, :], src=o_, sem=o_))

        c_s1(0)
        for i in range(16):
            bk = c_s2_pe(i)
            if i + 1 < 16:
                c_s1(i + 1)
            c_s2_rest(i, bk)

    P.emit()
    return nc


def _consts():
    c = np.zeros((128, 768), np.float32)
    c[:, 0:128] = np.eye(128, dtype=np.float32)
    c[:, 128:256] = np.triu(np.ones((128, 128), np.float32))
    c[127, 256:384] = 1.0
    c[:, 384:512] = 1.0
    c[:, 512:640] = np.triu(np.ones((128, 128), np.float32), k=1)
    c[:, 640:768] = np.arange(128, dtype=np.float32)[None, :]
    return c


def make_in_maps(inp, cores):
    f = lambda a: np.ascontiguousarray(a, dtype=np.float32)
    w_in = inp["w_in"][0]
    b_in = inp["b_in"][0]
    cq, ck, cv, cf, cu, cgv, cga, cgb = (np.arange(0, 512), np.arange(512, 1024), np.arange(1024, 1536),
                                         np.arange(1536, 1544), np.arange(1544, 2056), np.arange(2056, 2568),
                                         np.arange(2568, 3592), np.arange(3592, 4616))
    fm = np.concatenate([cq, ck, cu, cga, cgb])
    tm = np.concatenate([cv, cgv, cf])
    rep = lambda v: f(np.broadcast_to(v[None, :], (128, v.shape[0])))
    shared = {
        "w_fm": f(w_in[:, fm].reshape(8, 128, 3584)),
        "w_tm": f(w_in[:, tm].reshape(8, 128, 1032)),
        "b_fm": f(b_in[fm].reshape(28, 128).T),
        "b_tm": rep(b_in[tm]),
        "lng": rep(inp["gmlp_ln_g"][0]),
        "lnb": rep(inp["gmlp_ln_b"][0]),
        "wsT": f(inp["w_spatial"][0].transpose(2, 0, 1).reshape(128, 1024)),
        "bsp": f(inp["b_spatial"][0].reshape(1, 1024)),
        "w_a": f(inp["w_branch_a"][0].reshape(8, 64, 1024).transpose(1, 0, 2).reshape(64, 8192)),
        "w_b": f(inp["w_branch_b"][0].reshape(4, 128, 1024).transpose(1, 0, 2).reshape(128, 4096)),
        "w_o": f(inp["w_out"][0].reshape(8, 128, 1024).transpose(1, 0, 2).reshape(128, 8192)),
        "vecs": f(np.concatenate([rep(inp[k][0]) for k in
                                  ("b_out", "ln1_g", "ln1_b", "ln2_g", "ln2_b", "b_ple_gate", "ln3_g", "ln3_b")], axis=1)),
        "w_r": f(inp["w_router"][0].reshape(8, 128, 32).transpose(1, 0, 2).reshape(128, 256)),
        "b_r": rep(inp["b_router"][0]),
        "wgu": f(inp["w_gate_up"][0].reshape(NE, 8, 128, 2, 8, 128).transpose(0, 4, 2, 3, 1, 5).reshape(NE, 8, 128, 2048)),
        "bgu": f(inp["b_gate_up"][0].reshape(NE, 16, 128).transpose(2, 0, 1).reshape(128, NE * 16)),
        "wdn": f(inp["w_down"][0].reshape(NE, 8, 128, 1024).transpose(0, 2, 1, 3).reshape(NE, 128, 8192)),
        "bdn": f(inp["b_down"][0].reshape(NE, 1, 1024)),
        "w_pl": f(inp["w_ple"][0].reshape(2, 128, 1024).transpose(1, 0, 2).reshape(128, 2048)),
        "w_pg": f(inp["w_ple_gate"][0].reshape(8, 128, 1024).transpose(1, 0, 2).reshape(128, 8192)),
        "cst": _consts(),
    }
    maps = []
    for c in cores:
        xc = inp["x"][2 * c:2 * c + 2].reshape(TOK, D)
        pc = inp["p"][0, 2 * c:2 * c + 2].reshape(TOK, 256)
        m = dict(shared)
        m["x_tm"] = f(xc)
        m["xT"] = f(xc.T.reshape(8, 128, TOK))
        m["pT"] = f(pc.T.reshape(2, 128, TOK))
        maps.append(m)
    return maps


def kernel(**inputs):
    inp = {k: np.asarray(v) for k, v in inputs.items()}
    nc = build()
    maps = make_in_maps(inp, list(range(NCORES)))
    res = run_bass_kernel_spmd(nc, maps, core_ids=list(range(NCORES)))
    outs = [np.asarray(r["out"]).reshape(2, S, D) for r in res.results]
    return np.concatenate(outs, axis=0).astype(np.float32)
```
